# Optimizing a Trainium2 kernel written in Bass

```python
import math
import jax, jax.numpy as jnp
from jax import lax
import numpy as np

D_MODEL = 1024
BATCH = 8
SEQ = 4096
DEPTH = 2

HEAD_DIM = 64
A_HEADS = 6
A_PATTERNS = ((128, 1), (512, 4), (2048, 16))
B_HEADS = 5
B_QK_DIM = 32
B_V_DIM = 64
C_HEADS = 5
C_Q_RANK = 192
C_KV_RANK = 128
C_NOPE_DIM = 64
C_ROPE_DIM = 32
C_V_DIM = 64
C_ROPE_THETA = 10000.0
ROPE_THETA = 500000.0
ROPE_FRACTION = 4
IN_COLS = (A_HEADS * HEAD_DIM, A_HEADS * HEAD_DIM, A_HEADS * HEAD_DIM,
           B_HEADS * 2 * B_QK_DIM, B_HEADS * 2 * B_QK_DIM, B_HEADS * B_V_DIM,
           C_Q_RANK, C_KV_RANK, C_ROPE_DIM)
IN_WIDTH = sum(IN_COLS)
MIX_WIDTH = A_HEADS * HEAD_DIM + B_HEADS * B_V_DIM + C_HEADS * C_V_DIM
N_EXPERTS = 16
CAPACITY_FACTOR = 2
EXPERT_FF = 1408
Q_BLOCK = 128
NORM_EPS = 1e-6
SUBLN_EPS = 1e-5
NEG_INF = -1e30

kernel_name = 'hybrid_dilated_diff_mla_ec_encoder'


def rmsnorm(x, g, eps=NORM_EPS):
    xf = x.astype(jnp.float32)
    y = xf * lax.rsqrt(jnp.mean(xf * xf, axis=-1, keepdims=True) + eps)
    return (y * g.astype(jnp.float32)).astype(x.dtype)


def rope_angles(positions, rot_dim, theta):
    inv_freq = 1.0 / (theta ** (jnp.arange(0, rot_dim, 2, dtype=jnp.float32) / rot_dim))
    ang = positions.astype(jnp.float32)[:, :, None] * inv_freq
    return jnp.cos(ang)[:, :, None, :], jnp.sin(ang)[:, :, None, :]


def rotate(x, cos, sin):
    half = x.shape[-1] // 2
    x1 = x[..., :half].astype(jnp.float32)
    x2 = x[..., half:].astype(jnp.float32)
    return jnp.concatenate([x1 * cos - x2 * sin, x2 * cos + x1 * sin], axis=-1).astype(x.dtype)


def partial_rotate(x, cos, sin):
    r = 2 * cos.shape[-1]
    return jnp.concatenate([rotate(x[..., :r], cos, sin), x[..., r:]], axis=-1)


def dilated_window_stats(q, k, v, window, dilation):
    Bn, S, H, D = q.shape
    r = dilation
    n = window // (2 * dilation)
    L = S // r
    nb = -(-L // n)
    Lp = nb * n

    def residue_major(t):
        return t.reshape(Bn, L, r, H, D).transpose(0, 2, 3, 1, 4)

    qb = jnp.pad(residue_major(q), ((0, 0), (0, 0), (0, 0), (0, Lp - L), (0, 0))).reshape(Bn, r, H, nb, n, D)

    def key_windows(t):
        tp = jnp.pad(residue_major(t), ((0, 0), (0, 0), (0, 0), (n, Lp - L + n), (0, 0)))
        tp = tp.reshape(Bn, r, H, nb + 2, n, D)
        return jnp.concatenate([tp[:, :, :, :-2], tp[:, :, :, 1:-1], tp[:, :, :, 2:]], axis=4)

    kw = key_windows(k)
    vw = key_windows(v).astype(jnp.float32)
    s = jnp.einsum('brhiqd,brhikd->brhiqk', qb, kw, preferred_element_type=jnp.float32) * (D ** -0.5)
    qi = jnp.arange(n)[:, None]
    ki = jnp.arange(3 * n)[None, :]
    delta = ki - n - qi
    key_pos = jnp.arange(nb)[:, None, None] * n + ki[None] - n
    valid = (jnp.abs(delta) <= n)[None] & (key_pos >= 0) & (key_pos < L)
    s = jnp.where(valid, s, NEG_INF)
    m = jnp.max(s, axis=-1)
    p = jnp.exp(s - m[..., None])
    l = jnp.sum(p, axis=-1)
    o = jnp.einsum('brhiqk,brhikd->brhiqd', p, vw)

    def back(t):
        t = t.reshape((Bn, r, H, Lp) + t.shape[5:])[:, :, :, :L]
        t = jnp.moveaxis(t, 3, 1)
        return t.reshape((Bn, S, H) + t.shape[4:])

    return back(o), back(m), back(l)


def dilated_mixture_attention(q, k, v):
    stats = [dilated_window_stats(q, k, v, w, r) for (w, r) in A_PATTERNS]
    m_max = jnp.max(jnp.stack([st[1] for st in stats]), axis=0)
    num = 0.0
    den = 0.0
    for o, m, l in stats:
        e = jnp.exp(m - m_max)
        num = num + e[..., None] * o
        den = den + e * l
    return (num / den[..., None]).astype(q.dtype)


def to_blocks(t):
    Bn, S = t.shape[:2]
    return jnp.moveaxis(t.reshape((Bn, S // Q_BLOCK, Q_BLOCK) + t.shape[2:]), 1, 0)


def from_blocks(t):
    t = jnp.moveaxis(t, 0, 1)
    return t.reshape((t.shape[0], t.shape[1] * t.shape[2]) + t.shape[3:])


def differential_attention(q, k, v, lam):
    scale = q.shape[-1] ** -0.5
    vf = v.astype(jnp.float32)

    def one(qb):
        s = jnp.einsum('bqhcd,bkhcd->bhcqk', qb, k, preferred_element_type=jnp.float32) * scale
        p = jax.nn.softmax(s, axis=-1)
        w = p[:, :, 0] - lam * p[:, :, 1]
        return jnp.einsum('bhqk,bkhd->bqhd', w, vf)

    return from_blocks(lax.map(one, to_blocks(q))).astype(v.dtype)


def latent_attention(q_nope, q_rope, k_nope, k_rope, v):
    scale = (q_nope.shape[-1] + q_rope.shape[-1]) ** -0.5
    vf = v.astype(jnp.float32)

    def one(blk):
        qn, qr = blk
        s = (jnp.einsum('bqhd,bkhd->bhqk', qn, k_nope, preferred_element_type=jnp.float32)
             + jnp.einsum('bqhr,bkr->bhqk', qr, k_rope, preferred_element_type=jnp.float32)) * scale
        p = jax.nn.softmax(s, axis=-1)
        return jnp.einsum('bhqk,bkhd->bqhd', p, vf)

    return from_blocks(lax.map(one, (to_blocks(q_nope), to_blocks(q_rope)))).astype(v.dtype)


def hybrid_mixer(h, positions, w_in, lam_q1, lam_k1, lam_q2, lam_k2, diff_subln_g, lam_init,
                 q_norm_g, w_uq, kv_norm_g, w_ukv, a_out_g, c_out_g, w_out):
    Bn, S, _ = h.shape
    proj = jnp.einsum('bsd,de->bse', h, w_in)
    split_points = np.cumsum(IN_COLS)[:-1].tolist()
    aq, ak, av, bq, bk, bv, cq, ckv, ckr = jnp.split(proj, split_points, axis=-1)

    cos_a, sin_a = rope_angles(positions, HEAD_DIM // ROPE_FRACTION, ROPE_THETA)
    aq = partial_rotate(aq.reshape(Bn, S, A_HEADS, HEAD_DIM), cos_a, sin_a)
    ak = partial_rotate(ak.reshape(Bn, S, A_HEADS, HEAD_DIM), cos_a, sin_a)
    av = av.reshape(Bn, S, A_HEADS, HEAD_DIM)
    oa = dilated_mixture_attention(aq, ak, av)
    oa = rmsnorm(oa, a_out_g.reshape(A_HEADS, HEAD_DIM))

    cos_b, sin_b = rope_angles(positions, B_QK_DIM // ROPE_FRACTION, ROPE_THETA)
    bq = partial_rotate(bq.reshape(Bn, S, 2 * B_HEADS, B_QK_DIM), cos_b, sin_b).reshape(Bn, S, B_HEADS, 2, B_QK_DIM)
    bk = partial_rotate(bk.reshape(Bn, S, 2 * B_HEADS, B_QK_DIM), cos_b, sin_b).reshape(Bn, S, B_HEADS, 2, B_QK_DIM)
    bv = bv.reshape(Bn, S, B_HEADS, B_V_DIM)
    f32 = jnp.float32
    lam = (jnp.exp(jnp.sum(lam_q1.astype(f32) * lam_k1.astype(f32)))
           - jnp.exp(jnp.sum(lam_q2.astype(f32) * lam_k2.astype(f32))) + lam_init)
    ob = differential_attention(bq, bk, bv, lam)
    ob = rmsnorm(ob, diff_subln_g * (1.0 - lam_init), eps=SUBLN_EPS)

    cos_c, sin_c = rope_angles(positions, C_ROPE_DIM, C_ROPE_THETA)
    qc = jnp.einsum('bsr,re->bse', rmsnorm(cq, q_norm_g), w_uq).reshape(Bn, S, C_HEADS, C_NOPE_DIM + C_ROPE_DIM)
    q_nope = qc[..., :C_NOPE_DIM]
    q_rope = rotate(qc[..., C_NOPE_DIM:], cos_c, sin_c)
    kv = jnp.einsum('bsr,re->bse', rmsnorm(ckv, kv_norm_g), w_ukv).reshape(Bn, S, C_HEADS, C_NOPE_DIM + C_V_DIM)
    k_nope = kv[..., :C_NOPE_DIM]
    vc = kv[..., C_NOPE_DIM:]
    k_rope = rotate(ckr[:, :, None, :], cos_c, sin_c)[:, :, 0]
    oc = latent_attention(q_nope, q_rope, k_nope, k_rope, vc)
    oc = rmsnorm(oc, c_out_g.reshape(C_HEADS, C_V_DIM))

    o = jnp.concatenate([oa.reshape(Bn, S, -1), ob.reshape(Bn, S, -1), oc.reshape(Bn, S, -1)], axis=-1)
    return jnp.einsum('bse,ed->bsd', o, w_out)


def expert_choice_ffn(h, w_router, w_gate, w_up, w_down):
    Bn, S, D = h.shape
    cap = CAPACITY_FACTOR * S // N_EXPERTS
    logits = jnp.einsum('bsd,de->bse', h, w_router, preferred_element_type=jnp.float32)
    affinity = jax.nn.softmax(logits, axis=-1)
    gate, idx = lax.top_k(jnp.swapaxes(affinity, 1, 2), cap)
    xe = jax.vmap(lambda hb, ib: hb[ib])(h, idx)
    a = jnp.einsum('becd,edf->becf', xe, w_gate)
    u = jnp.einsum('becd,edf->becf', xe, w_up)
    y = jnp.einsum('becf,efd->becd', jax.nn.silu(a) * u, w_down)
    y = y * gate[..., None].astype(y.dtype)

    def scatter(yb, ib):
        return jnp.zeros((S, D), yb.dtype).at[ib.reshape(-1)].add(yb.reshape(-1, D))

    return jax.vmap(scatter)(y, idx)


def setup_inputs(seed: int = 0) -> dict:
    key = jax.random.key(seed)
    ks = jax.random.split(key, 24)
    f32 = jnp.float32

    def nrm(k, shape, fan_in):
        return jax.random.normal(k, shape, f32) * (fan_in ** -0.5)

    def gain(k, shape):
        return 1.0 + 0.02 * jax.random.normal(k, shape, f32)

    x = jax.random.normal(ks[0], (BATCH, SEQ, D_MODEL), f32)
    offsets = jax.random.randint(ks[1], (BATCH, 1), 0, 1024)
    positions = (jnp.arange(SEQ, dtype=jnp.int32)[None, :] + offsets).astype(jnp.int32)
    return {
        'x': x,
        'positions': positions,
        'attn_norm_g': gain(ks[2], (DEPTH, D_MODEL)),
        'w_in': nrm(ks[3], (DEPTH, D_MODEL, IN_WIDTH), D_MODEL),
        'lam_q1': 0.1 * jax.random.normal(ks[4], (DEPTH, B_QK_DIM), f32),
        'lam_k1': 0.1 * jax.random.normal(ks[5], (DEPTH, B_QK_DIM), f32),
        'lam_q2': 0.1 * jax.random.normal(ks[6], (DEPTH, B_QK_DIM), f32),
        'lam_k2': 0.1 * jax.random.normal(ks[7], (DEPTH, B_QK_DIM), f32),
        'diff_subln_g': gain(ks[8], (DEPTH, B_V_DIM)),
        'mla_q_norm_g': gain(ks[9], (DEPTH, C_Q_RANK)),
        'mla_w_uq': nrm(ks[10], (DEPTH, C_Q_RANK, C_HEADS * (C_NOPE_DIM + C_ROPE_DIM)), C_Q_RANK),
        'mla_kv_norm_g': gain(ks[11], (DEPTH, C_KV_RANK)),
        'mla_w_ukv': nrm(ks[12], (DEPTH, C_KV_RANK, C_HEADS * (C_NOPE_DIM + C_V_DIM)), C_KV_RANK),
        'dil_out_g': gain(ks[13], (DEPTH, A_HEADS * HEAD_DIM)),
        'mla_out_g': gain(ks[14], (DEPTH, C_HEADS * C_V_DIM)),
        'w_out': nrm(ks[15], (DEPTH, MIX_WIDTH, D_MODEL), MIX_WIDTH),
        'ffn_norm_g': gain(ks[16], (DEPTH, D_MODEL)),
        'w_router': nrm(ks[17], (DEPTH, D_MODEL, N_EXPERTS), D_MODEL),
        'w_gate': nrm(ks[18], (DEPTH, N_EXPERTS, D_MODEL, EXPERT_FF), D_MODEL),
        'w_up': nrm(ks[19], (DEPTH, N_EXPERTS, D_MODEL, EXPERT_FF), D_MODEL),
        'w_down': nrm(ks[20], (DEPTH, N_EXPERTS, EXPERT_FF, D_MODEL), EXPERT_FF),
        'final_norm_g': gain(ks[21], (D_MODEL,)),
    }


def reference(x, positions, attn_norm_g, w_in, lam_q1, lam_k1, lam_q2, lam_k2, diff_subln_g,
              mla_q_norm_g, mla_w_uq, mla_kv_norm_g, mla_w_ukv, dil_out_g, mla_out_g, w_out,
              ffn_norm_g, w_router, w_gate, w_up, w_down, final_norm_g):
    for l in range(DEPTH):
        lam_init = 0.8 - 0.6 * math.exp(-0.3 * l)
        h = rmsnorm(x, attn_norm_g[l])
        x = x + hybrid_mixer(h, positions, w_in[l], lam_q1[l], lam_k1[l], lam_q2[l], lam_k2[l],
                             diff_subln_g[l], lam_init, mla_q_norm_g[l], mla_w_uq[l],
                             mla_kv_norm_g[l], mla_w_ukv[l], dil_out_g[l], mla_out_g[l], w_out[l])
        h = rmsnorm(x, ffn_norm_g[l])
        x = x + expert_choice_ffn(h, w_router[l], w_gate[l], w_up[l], w_down[l])
    return rmsnorm(x, final_norm_g)
```

```python
import math
import numpy as np
import ml_dtypes
import concourse.bass as bass
import concourse.mybir as mybir
from concourse.bass_utils import run_bass_kernel_spmd

F32 = mybir.dt.float32
BF16 = mybir.dt.bfloat16
I32 = mybir.dt.int32
AF = mybir.ActivationFunctionType
ALU = mybir.AluOpType

S = 4096
D = 1024
NT = 32
DEPTH = 2
INW = 2464
NE = 16
CAP = 512
FF = 1408
NF = 11
NDMASEM = 48


class Unit:
    __slots__ = ("last_w", "rd_eng", "rd_dma")

    def __init__(self):
        self.last_w = None
        self.rd_eng = {}
        self.rd_dma = []


class Op:
    __slots__ = ("eng", "fn", "deps", "is_dma", "signal", "sem", "val", "epoch")

    def __init__(self, eng, fn, is_dma):
        self.eng = eng
        self.fn = fn
        self.deps = []
        self.is_dma = is_dma
        self.signal = False
        self.sem = None
        self.val = 0
        self.epoch = 0


class T:
    def __init__(self, h):
        self.h = h
        self.u = Unit()

    def __getitem__(self, k):
        return self.h[k]

    def ap(self):
        return self.h.ap()


class Bank:
    def __init__(self, ap):
        self.a = ap
        self.u = Unit()

    def __getitem__(self, k):
        return self.a[k]


class Prog:
    def __init__(self, nc):
        self.nc = nc
        self.ops = []
        self.engs = {"pe": nc.tensor, "act": nc.scalar, "dve": nc.vector, "pool": nc.gpsimd, "sp": nc.sync}
        self.uid = 0
        self.epoch = 0
        self.last_eng = {}
        self.open_dma = []
        self.es = None
        self.st = None

    def sb(self, shape, dtype):
        self.uid += 1
        return T(self.es.enter_context(self.nc.sbuf_tensor(f"sb{self.uid}", list(shape), dtype)))

    def ps_all(self):
        return self.nc.alloc_psum_tensor("psall", [128, 4096], F32)

    def dram(self, name, shape, dtype, kind="Internal"):
        return T(self.nc.dram_tensor(name, list(shape), dtype, kind=kind))

    def _dep(self, op, d):
        if d is None or d is op:
            return
        if not (op.is_dma or d.is_dma) and d.eng == "pe" and op.eng == "pe":
            return
        if d not in op.deps:
            op.deps.append(d)
            d.signal = True

    def op(self, eng, fn, r=(), w=(), dma=False):
        o = Op(eng, fn, dma)
        o.epoch = self.epoch
        us_r = [t.u if hasattr(t, 'u') else t for t in r]
        us_w = [t.u if hasattr(t, 'u') else t for t in w]
        for u in us_r:
            self._dep(o, u.last_w)
        for u in us_w:
            lw = u.last_w
            if lw is not None and (o.is_dma or lw.is_dma or lw.eng != o.eng):
                self._dep(o, lw)
            for e, rd in u.rd_eng.items():
                if o.is_dma or e != o.eng:
                    self._dep(o, rd)
            for rd in u.rd_dma:
                self._dep(o, rd)
        for u in us_r:
            if dma:
                u.rd_dma.append(o)
            else:
                u.rd_eng[eng] = o
        for u in us_w:
            u.last_w = o
            u.rd_eng = {}
            u.rd_dma = []
        self.ops.append(o)
        if dma:
            self.open_dma.append(o)
        else:
            self.last_eng[eng] = o
        return o

    def barrier(self):
        pend = list(self.last_eng.values()) + self.open_dma[-NDMASEM:]
        self.open_dma = self.open_dma[-NDMASEM:]
        news = []
        for eng in ("pe", "act", "dve", "pool", "sp"):
            o = Op(eng, lambda e: e.nop(), False)
            o.epoch = self.epoch
            for d in pend:
                if d.eng == eng and not d.is_dma:
                    continue
                o.deps.append(d)
                d.signal = True
            self.ops.append(o)
            news.append(o)
        self.epoch += 1
        self.last_eng = {}

    def dma(self, q, out, in_, r, w):
        return self.op(q, lambda e: e.dma_start(out=out, in_=in_), r=r, w=w, dma=True)

    def mm(self, out, lhsT, rhs, start, stop, r, w):
        return self.op("pe", lambda e: e.matmul(out, lhsT=lhsT, rhs=rhs, start=start, stop=stop), r=r, w=w)

    def tr(self, out, in_, ident, r, w):
        return self.op("pe", lambda e: e.transpose(out, in_, ident), r=r, w=w)

    def act(self, out, in_, func, r, w, **kw):
        return self.op("act", lambda e: e.activation(out=out, in_=in_, func=func, **kw), r=r, w=w)

    def cp(self, eng, out, in_, r, w):
        if eng == "act":
            return self.op("act", lambda e: e.copy(out=out, in_=in_), r=r, w=w)
        return self.op(eng, lambda e: e.tensor_copy(out=out, in_=in_), r=r, w=w)

    def tt(self, eng, out, in0, in1, op, r, w):
        return self.op(eng, lambda e: e.tensor_tensor(out=out, in0=in0, in1=in1, op=op), r=r, w=w)

    def ts(self, eng, out, in0, s1, s2, op0, op1, r, w, **kw):
        return self.op(eng, lambda e: e.tensor_scalar(out=out, in0=in0, scalar1=s1, scalar2=s2, op0=op0, op1=op1, **kw), r=r, w=w)

    def stt(self, out, in0, scalar, in1, op0, op1, r, w):
        return self.op("dve", lambda e: e.scalar_tensor_tensor(out=out, in0=in0, scalar=scalar, in1=in1, op0=op0, op1=op1), r=r, w=w)

    def memset(self, eng, ap, val, w):
        return self.op(eng, lambda e: e.memset(ap, val), w=w)

    def emit(self):
        nc = self.nc
        if self.st is None:
            self.st = dict(esems={}, dsem=[nc.alloc_semaphore(f"ds{i}") for i in range(NDMASEM)], cnt={},
                           seen={k: {} for k in self.engs}, ndma=0)
        st = self.st
        esems, dsem, cnt, seen, ndma = st["esems"], st["dsem"], st["cnt"], st["seen"], st["ndma"]
        ops, self.ops = self.ops, []
        for o in ops:
            e = self.engs[o.eng]
            sn = seen[o.eng]
            for d in o.deps:
                key = id(d.sem)
                if sn.get(key, 0) < d.val:
                    e.wait_ge(d.sem, d.val)
                    sn[key] = d.val
            if o.is_dma:
                s = dsem[ndma % NDMASEM]
                v = 16 * (ndma // NDMASEM + 1)
                ndma += 1
                key = id(s)
                if v > 16 and sn.get(key, 0) < v - 16:
                    e.wait_ge(s, v - 16)
                    sn[key] = v - 16
                ins = o.fn(e)
                ins.then_inc(s, 16)
                o.sem, o.val = s, v
            else:
                ins = o.fn(e)
                if o.signal:
                    k = (o.eng, o.epoch)
                    if k not in esems:
                        esems[k] = nc.alloc_semaphore(f"es_{o.eng}_{o.epoch}")
                        cnt[k] = 0
                    cnt[k] += 1
                    o.sem, o.val = esems[k], cnt[k]
                    ins.then_inc(o.sem, 1)
        st["ndma"] = ndma
        return ndma


def bc(ap, shape):
    return ap.broadcast_to(list(shape))


def _define(es_glob, debug):
    nc = bass.Bass("TRN2", target_bir_lowering=False)
    P = Prog(nc)
    P.es = es_glob
    kin = "ExternalInput"
    kdbg = "ExternalOutput" if debug else "Internal"
    x_in = P.dram("x", [S, D], F32, kin)
    pos_in = P.dram("pos", [128, NT], I32, kin)
    win_in = P.dram("win", [DEPTH, 128, 8, INW], F32, kin)
    wout_in = P.dram("wout", [DEPTH, 128, 8, D], F32, kin)
    wuq_in = P.dram("wuq", [DEPTH, 192, 480], F32, kin)
    wukv_in = P.dram("wukv", [DEPTH, 128, 640], F32, kin)
    wr_in = P.dram("wr", [DEPTH, 128, 8, NE], F32, kin)
    nle = 1 if debug == "noexp" else DEPTH
    nee = 1 if debug == "noexp" else NE
    wg_in = P.dram("wg", [nle, nee, NF, 128, D], F32, kin)
    wu_in = P.dram("wu", [nle, nee, NF, 128, D], F32, kin)
    wd_in = P.dram("wd", [nle, nee, FF, D], F32, kin)
    grow_in = P.dram("grow", [DEPTH, 2, D], F32, kin)
    fing_in = P.dram("fing", [1, D], F32, kin)
    qng_in = P.dram("qng", [DEPTH, 192], F32, kin)
    kvng_in = P.dram("kvng", [DEPTH, 128], F32, kin)
    hg_in = P.dram("hg", [DEPTH, 64, 12], F32, kin)
    lam_in = P.dram("lam", [DEPTH, 4, 32], F32, kin)
    identb_in = P.dram("identb", [128, 128], BF16, kin)
    identf_in = P.dram("identf", [128, 128], F32, kin)
    invf_in = P.dram("invf", [128, 28], F32, kin)
    amask_in = P.dram("amask", [128, 20, 512], BF16, kin)
    tri_in = P.dram("tri", [128, 128], BF16, kin)
    gsum_in = P.dram("gsum", [128, 128], F32, kin)
    iota_in = P.dram("iota", [128, 512], mybir.dt.float16, kin)
    tokab_in = P.dram("tokab", [128, NT, 2], BF16, kin)
    out_d = P.dram("out", [S, D], F32, "ExternalOutput")
    QKA = P.dram("QKA", [768, S], BF16, kdbg)
    QKB = P.dram("QKB", [640, S], BF16, kdbg)
    QKC = P.dram("QKC", [10, 96, S], BF16, kdbg)
    VALL = P.dram("VALL", [S, D], BF16, kdbg)
    OT = P.dram("OT", [D, S], BF16, kdbg)
    XR = P.dram("XR", [S, D], F32, kdbg)
    H = P.dram("H", [S, D], BF16, kdbg)
    THR = P.dram("THR", [NE, 1], F32, kdbg)
    AFF = P.dram("AFF", [128, NT * NE], F32, kdbg)
    IDXD = P.dram("IDXD", [128, 64 * 2], F32, kdbg)

    PSALL = P.ps_all()
    psum = [Bank(PSALL[:, i * 512:(i + 1) * 512]) for i in range(8)]

    def pbf(i):
        return psum[i].a.bitcast(BF16)

    identb = P.sb([128, 128], BF16)
    identf = P.sb([128, 128], F32)
    P.dma("sp", identb[:], identb_in[:, :], [identb_in], [identb])
    P.dma("sp", identf[:], identf_in[:, :], [identf_in], [identf])
    sin_all = P.sb([128, NT, 28], F32)
    cos_all = P.sb([128, NT, 28], F32)


    def phase0():
        invf = P.sb([128, 28], F32)
        posi = P.sb([128, NT], I32)
        posf = P.sb([128, NT], F32)
        tt_ = P.sb([128, NT, 28], F32)
        ki = P.sb([128, NT, 28], I32)
        kf = P.sb([128, NT, 28], F32)
        fr = P.sb([128, NT, 28], F32)
        P.dma("sp", invf[:], invf_in[:, :], [invf_in], [invf])
        P.dma("sp", posi[:], pos_in[:, :], [pos_in], [posi])
        P.cp("dve", posf[:], posi[:], [posi], [posf])
        P.tt("dve", tt_[:], bc(posf[:].unsqueeze(2), [128, NT, 28]), bc(invf[:].unsqueeze(1), [128, NT, 28]), ALU.mult, [posf, invf], [tt_])
        for dst, shift in ((sin_all, 0.0), (cos_all, 0.25)):
            if shift:
                P.ts("dve", tt_[:], tt_[:], shift, None, ALU.add, ALU.bypass, [tt_], [tt_])
            P.cp("dve", ki[:], tt_[:], [tt_], [ki])
            P.cp("dve", kf[:], ki[:], [ki], [kf])
            P.tt("dve", fr[:], tt_[:], kf[:], ALU.subtract, [tt_, kf], [fr])
            P.ts("dve", fr[:], fr[:], 0.5, -0.5, ALU.min, ALU.max, [fr], [fr])
            P.act(dst[:], fr[:], AF.Sin, [fr], [dst], scale=2.0 * math.pi)

    def phase1(l, x_src):
        win_bf = P.sb([128, 8, INW], BF16)
        for c in range(8):
            for h_ in range(2):
                P.dma("pool", win_bf[:, c, h_ * 1232:(h_ + 1) * 1232], win_in[l, :, c, h_ * 1232:(h_ + 1) * 1232], [win_in], [win_bf])
        wuq_f = P.sb([128, 2, 480], F32)
        wuq_bf = P.sb([128, 2, 480], BF16)
        wukv_f = P.sb([128, 640], F32)
        wukv_bf = P.sb([128, 640], BF16)
        P.dma("sp", wuq_f[:, 0, :], wuq_in[l, 0:128, :], [wuq_in], [wuq_f])
        P.dma("sp", wuq_f[0:64, 1, :], wuq_in[l, 128:192, :], [wuq_in], [wuq_f])
        P.dma("sp", wukv_f[:], wukv_in[l, :, :], [wukv_in], [wukv_f])
        P.cp("dve", wuq_bf[:, 0, :], wuq_f[:, 0, :], [wuq_f], [wuq_bf])
        P.cp("dve", wuq_bf[0:64, 1, :], wuq_f[0:64, 1, :], [wuq_f], [wuq_bf])
        P.cp("dve", wukv_bf[:], wukv_f[:], [wukv_f], [wukv_bf])
        g_attn = P.sb([128, D], F32)
        g_qn = P.sb([128, 192], F32)
        g_kvn = P.sb([128, 128], F32)
        P.dma("sp", g_attn[:], grow_in[l, 0:1, :].partition_broadcast(128), [grow_in], [g_attn])
        P.dma("sp", g_qn[:], qng_in[l:l + 1, :].partition_broadcast(128), [qng_in], [g_qn])
        P.dma("sp", g_kvn[:], kvng_in[l:l + 1, :].partition_broadcast(128), [kvng_in], [g_kvn])

        def mk(shape, dt):
            return [P.sb(shape, dt) for _ in range(2)]
        xt = [P.sb([128, D], F32) for _ in range(4)]; junk = mk([128, D], BF16); xn = mk([128, D], BF16); hT = mk([128, D], BF16)
        st1 = mk([128, 8], F32)
        st2 = mk([128, 8], F32)
        pr = mk([128, INW], F32)
        qkA = mk([128, 768], BF16); qkB = mk([128, 640], BF16); vst = mk([128, D], BF16)
        rt = mk([128, 4, 160], F32)
        cqn = mk([128, 192], BF16); ckvn = mk([128, 128], BF16); cT = mk([128, 384], BF16)
        qcs = mk([128, 480], F32); kvs = mk([128, 640], F32)
        QCb = mk([128, 5, 96], BF16); KCb = mk([128, 5, 96], BF16); kr = mk([128, 32], F32)
        stA = mk([128, 768], BF16); stB = mk([128, 640], BF16); stC = mk([128, 10, 128], BF16)
        x_rows = x_src.ap().rearrange("(j p) d -> j p d", p=128)
        QKA_v = QKA.ap().rearrange("(c p) t -> p c t", p=128)
        QKB_v = QKB.ap().rearrange("(c p) t -> p c t", p=128)
        QKC_v = QKC.ap().rearrange("i r t -> r i t")
        V_rows = VALL.ap().rearrange("(j p) d -> j p d", p=128)
        T0, T1, PJ0, PJ1, PQ, PK0, PK1 = 0, 1, 2, 3, 4, 5, 6

        def rms(src_ap, n, eps, stt_, c0, r, pa):
            P.act(junk[pa][:, 0:n], src_ap, AF.Square, r, [junk[pa], stt_], accum_out=stt_[:, c0:c0 + 1])
            P.act(stt_[:, c0 + 1:c0 + 2], stt_[:, c0:c0 + 1], AF.Ln, [stt_], [stt_], scale=1.0 / n, bias=eps)
            P.act(stt_[:, c0 + 2:c0 + 3], stt_[:, c0 + 1:c0 + 2], AF.Exp, [stt_], [stt_], scale=-0.5)

        def rope(src3, dst3, lo, half, fo, nf, j, pa, rd, wr_, tmp):
            G = src3.shape[1]
            x1 = src3[:, :, lo:lo + half]; x2 = src3[:, :, lo + half:lo + 2 * half]
            cs = bc(cos_all[:, j, fo:fo + nf].unsqueeze(1), [128, G, half])
            sn = bc(sin_all[:, j, fo:fo + nf].unsqueeze(1), [128, G, half])
            n = G * half
            ta = tmp[:, 0, 0:n].rearrange("p (g k) -> p g k", k=half)
            tb = tmp[:, 1, 0:n].rearrange("p (g k) -> p g k", k=half)
            tc = tmp[:, 2, 0:n].rearrange("p (g k) -> p g k", k=half)
            td = tmp[:, 3, 0:n].rearrange("p (g k) -> p g k", k=half)
            P.tt("dve", ta, x1, cs, ALU.mult, rd + [cos_all], [tmp])
            P.tt("dve", tb, x2, sn, ALU.mult, rd + [sin_all], [tmp])
            P.tt("dve", tc, x2, cs, ALU.mult, rd + [cos_all], [tmp])
            P.tt("dve", td, x1, sn, ALU.mult, rd + [sin_all], [tmp])
            P.tt("dve", dst3[:, :, lo:lo + half], ta, tb, ALU.subtract, [tmp], wr_)
            P.tt("dve", dst3[:, :, lo + half:lo + 2 * half], tc, td, ALU.add, [tmp], wr_)

        def load_x(j):
            if j < NT:
                P.dma("sp", xt[j % 4][:], x_rows[j], [x_src], [xt[j % 4]])

        def s0(j):
            pa = j % 2
            x4 = xt[j % 4]
            rms(x4[:], D, 1e-6, st1[pa], 0, [x4], pa)
            yield
            P.stt(xn[pa][:], x4[:], st1[pa][:, 2:3], g_attn[:], ALU.mult, ALU.mult, [x4, st1[pa], g_attn], [xn[pa]])
            yield

        def s1f(j):
            pa = j % 2
            tb = pbf(T0)
            for c in range(8):
                P.tr(tb[:, c * 128:(c + 1) * 128], xn[pa][:, c * 128:(c + 1) * 128], identb[:], [xn[pa], identb], [psum[T0]])
            P.cp("act", hT[pa][:, 0:512], tb[:, 0:512], [psum[T0]], [hT[pa]])
            P.cp("dve", hT[pa][:, 512:1024], tb[:, 512:1024], [psum[T0]], [hT[pa]])

        def s1b(j, feed):
            pa = j % 2
            col = 0
            for cc in range(5):
                n = min(512, INW - col)
                bk = (PJ0, PJ1, 7)[cc % 3]
                for c in range(8):
                    P.mm(psum[bk][:, 0:n], hT[pa][:, c * 128:(c + 1) * 128], win_bf[:, c, col:col + n], c == 0, c == 7, [hT[pa], win_bf], [psum[bk]])
                feed(4)
                P.cp("act" if cc % 2 == 0 else "dve", pr[pa][:, col:col + n], psum[bk][:, 0:n], [psum[bk]], [pr[pa]])
                col += n

        def s2a(j):
            pa = j % 2
            p_ = pr[pa]
            P.cp("act", qkA[pa][:], p_[:, 0:768], [p_], [qkA[pa]])
            yield
            P.cp("pool", qkB[pa][:], p_[:, 1152:1792], [p_], [qkB[pa]])
            yield
            rope(p_[:, 0:768].rearrange("p (g k) -> p g k", k=64), qkA[pa][:].rearrange("p (g k) -> p g k", k=64), 0, 8, 0, 8, j, pa, [p_], [qkA[pa]], rt[pa])
            yield
            rope(p_[:, 1152:1792].rearrange("p (g k) -> p g k", k=32), qkB[pa][:].rearrange("p (g k) -> p g k", k=32), 0, 4, 8, 4, j, pa, [p_], [qkB[pa]], rt[pa])
            yield
            P.cp("pool", vst[pa][:, 0:384], p_[:, 768:1152], [p_], [vst[pa]])
            yield
            P.cp("pool", vst[pa][:, 384:704], p_[:, 1792:2112], [p_], [vst[pa]])
            yield
            rms(p_[:, 2112:2304], 192, 1e-6, st2[pa], 0, [p_], pa)
            yield
            P.stt(cqn[pa][:], p_[:, 2112:2304], st2[pa][:, 2:3], g_qn[:], ALU.mult, ALU.mult, [p_, st2[pa], g_qn], [cqn[pa]])
            yield
            rms(p_[:, 2304:2432], 128, 1e-6, st2[pa], 3, [p_], pa)
            yield
            P.stt(ckvn[pa][:], p_[:, 2304:2432], st2[pa][:, 5:6], g_kvn[:], ALU.mult, ALU.mult, [p_, st2[pa], g_kvn], [ckvn[pa]])
            yield
            rope(p_[:, 2432:2464].unsqueeze(1), kr[pa][:].unsqueeze(1), 0, 16, 12, 16, j, pa, [p_], [kr[pa]], rt[pa])
            yield

        def s2b(j):
            pa = j % 2
            tA = pbf(T1)
            for c in range(6):
                P.tr(tA[:, c * 128:(c + 1) * 128], qkA[pa][:, c * 128:(c + 1) * 128], identb[:], [qkA[pa], identb], [psum[T1]])
            P.cp("act", stA[pa][:], tA[:, 0:768], [psum[T1]], [stA[pa]])
            P.dma("sp", QKA_v[:, :, j * 128:(j + 1) * 128], stA[pa][:].rearrange("p (c t) -> p c t", t=128), [stA[pa]], [QKA])
            tB = pbf(T0)
            for c in range(5):
                P.tr(tB[:, c * 128:(c + 1) * 128], qkB[pa][:, c * 128:(c + 1) * 128], identb[:], [qkB[pa], identb], [psum[T0]])
            P.cp("dve", stB[pa][:], tB[:, 0:640], [psum[T0]], [stB[pa]])
            P.dma("sp", QKB_v[:, :, j * 128:(j + 1) * 128], stB[pa][:].rearrange("p (c t) -> p c t", t=128), [stB[pa]], [QKB])
            tM = pbf(T1)
            P.tr(tM[:, 0:128], cqn[pa][:, 0:128], identb[:], [cqn[pa], identb], [psum[T1]])
            P.tr(tM[0:64, 128:256], cqn[pa][:, 128:192], identb[:], [cqn[pa], identb], [psum[T1]])
            P.tr(tM[:, 256:384], ckvn[pa][:], identb[:], [ckvn[pa], identb], [psum[T1]])
            P.cp("act", cT[pa][:, 0:128], tM[:, 0:128], [psum[T1]], [cT[pa]])
            P.cp("act", cT[pa][0:64, 128:256], tM[0:64, 128:256], [psum[T1]], [cT[pa]])
            P.cp("act", cT[pa][:, 256:384], tM[:, 256:384], [psum[T1]], [cT[pa]])
            P.mm(psum[PQ][:, 0:480], cT[pa][:, 0:128], wuq_bf[:, 0, :], True, False, [cT[pa], wuq_bf], [psum[PQ]])
            P.mm(psum[PQ][:, 0:480], cT[pa][0:64, 128:256], wuq_bf[0:64, 1, :], False, True, [cT[pa], wuq_bf], [psum[PQ]])
            P.mm(psum[PK0][:, 0:320], cT[pa][:, 256:384], wukv_bf[:, 0:320], True, True, [cT[pa], wukv_bf], [psum[PK0]])
            P.mm(psum[PK1][:, 0:320], cT[pa][:, 256:384], wukv_bf[:, 320:640], True, True, [cT[pa], wukv_bf], [psum[PK1]])
            P.cp("dve", qcs[pa][:], psum[PQ][:, 0:480], [psum[PQ]], [qcs[pa]])
            P.cp("act", kvs[pa][:, 0:320], psum[PK0][:, 0:320], [psum[PK0]], [kvs[pa]])
            P.cp("dve", kvs[pa][:, 320:640], psum[PK1][:, 0:320], [psum[PK1]], [kvs[pa]])

        def s2c_pre(j):
            pa = j % 2
            q3 = qcs[pa][:].rearrange("p (g k) -> p g k", k=96)
            P.cp("pool", QCb[pa][:], q3, [qcs[pa]], [QCb[pa]])
            yield
            rope(q3, QCb[pa][:], 64, 16, 12, 16, j, pa, [qcs[pa]], [QCb[pa]], rt[pa])
            yield
            kv3 = kvs[pa][:].rearrange("p (g k) -> p g k", k=128)
            P.cp("pool", KCb[pa][:, :, 0:64], kv3[:, :, 0:64], [kvs[pa]], [KCb[pa]])
            yield
            P.cp("act", vst[pa][:, 704:1024].rearrange("p (g k) -> p g k", k=64), kv3[:, :, 64:128], [kvs[pa]], [vst[pa]])
            yield
            P.cp("dve", KCb[pa][:, :, 64:96], bc(kr[pa][:].unsqueeze(1), [128, 5, 32]), [kr[pa]], [KCb[pa]])
            yield
            P.dma("sp", V_rows[j], vst[pa][:], [vst[pa]], [VALL])
            yield

        def s2c_post(j):
            pa = j % 2
            tC = pbf(T0)
            for h in range(5):
                P.tr(tC[0:96, h * 128:(h + 1) * 128], QCb[pa][:, h, :], identb[:], [QCb[pa], identb], [psum[T0]])
            P.cp("act", stC[pa][0:96, 0:5, :], tC[0:96, 0:640].rearrange("p (c t) -> p c t", t=128), [psum[T0]], [stC[pa]])
            tD = pbf(T1)
            for h in range(5):
                P.tr(tD[0:96, h * 128:(h + 1) * 128], KCb[pa][:, h, :], identb[:], [KCb[pa], identb], [psum[T1]])
            P.cp("dve", stC[pa][0:96, 5:10, :], tD[0:96, 0:640].rearrange("p (c t) -> p c t", t=128), [psum[T1]], [stC[pa]])
            P.dma("sp", QKC_v[:, :, j * 128:(j + 1) * 128], stC[pa][0:96, :, :], [stC[pa]], [QKC])

        load_x(0)
        load_x(1)
        for step in range(NT + 3):
            load_x(step + 2)
            if 0 <= step - 1 < NT:
                s1f(step - 1)
            gens = []
            if step < NT:
                gens.append(s0(step))
            if 0 <= step - 2 < NT:
                gens.append(s2a(step - 2))
            if 0 <= step - 3 < NT:
                gens.append(s2c_pre(step - 3))

            def feed(k, gens=gens):
                while k > 0 and gens:
                    try:
                        next(gens[0])
                        k -= 1
                    except StopIteration:
                        gens.pop(0)

            if 0 <= step - 1 < NT:
                s1b(step - 1, feed)
            feed(10 ** 6)
            if 0 <= step - 2 < NT:
                s2b(step - 2)
            if 0 <= step - 3 < NT:
                s2c_post(step - 3)

    def phase2(l):
        lam_init = 0.8 - 0.6 * math.exp(-0.3 * l)
        hg = P.sb([64, 12], F32)
        P.dma("sp", hg[:], hg_in[l, :, :], [hg_in], [hg])
        gB = P.sb([64, 1], F32)
        P.ts("dve", gB[:], hg[:, 11:12], 1.0 - lam_init, None, ALU.mult, ALU.bypass, [hg], [gB])
        lp = P.sb([64, 4, 32], F32)
        for i in range(4):
            P.dma("sp", lp[:, i, :], lam_in[l, i:i + 1, :].partition_broadcast(64), [lam_in], [lp])
        lpr = P.sb([64, 2, 32], F32)
        lsm = P.sb([64, 8], F32)
        P.tt("dve", lpr[:, 0, :], lp[:, 0, :], lp[:, 1, :], ALU.mult, [lp], [lpr])
        P.tt("dve", lpr[:, 1, :], lp[:, 2, :], lp[:, 3, :], ALU.mult, [lp], [lpr])
        P.op("dve", lambda e: e.tensor_reduce(out=lsm[:, 0:2], in_=lpr[:], axis=mybir.AxisListType.X, op=ALU.add), r=[lpr], w=[lsm])
        P.act(lsm[:, 2:4], lsm[:, 0:2], AF.Exp, [lsm], [lsm])
        P.tt("dve", lsm[:, 4:5], lsm[:, 3:4], lsm[:, 2:3], ALU.subtract, [lsm], [lsm])
        P.ts("dve", lsm[:, 5:6], lsm[:, 4:5], -lam_init, None, ALU.add, ALU.bypass, [lsm], [lsm])
        neglam = lsm[:, 5:6]
        m128 = P.sb([128, 64], F32)
        P.memset("dve", m128[0:64, :], 1.0 / 64, [m128])
        P.memset("dve", m128[64:128, :], 1e-6 / 64, [m128])
        amask = P.sb([128, 20, 512], BF16)
        P.dma("sp", amask[:], amask_in[:, :, :], [amask_in], [amask])

        QT = [P.sb([128, S], BF16) for _ in range(2)]
        KT = [P.sb([128, S], BF16) for _ in range(2)]
        VV = [P.sb([128, NT, 128], BF16) for _ in range(2)]
        for v in VV:
            P.memset("pool", v[:], 1.0, [v])
        Qz = [[P.sb([128, 512], BF16) for _ in range(2)] for _ in range(6)]
        for zz in Qz:
            for z in zz:
                P.memset("pool", z[:], 0.0, [z])
        Pt = [P.sb([128, 1536], BF16) for _ in range(4)]
        Pm = [P.sb([128, 1536], BF16) for _ in range(4)]
        fin = [[P.sb([128, 512], F32) for _ in range(5)] for _ in range(2)]
        yb = [P.sb([64, 512], BF16) for _ in range(2)]
        RB_ = 0
        fe = [[P.sb([128, 512], F32) for _ in range(2)] for _ in range(2)]
        V_v = VALL.ap().rearrange("(k p) c -> p k c", p=128)

        heads = [("A", h) for h in range(6)] + [("B", h) for h in range(5)] + [("C", h) for h in range(5)]
        blk_of = []
        for kind, h in heads:
            blk_of.append((kind, h if kind == "C" else h // 2))
        blk_seq = []
        for b_ in blk_of:
            if not blk_seq or blk_seq[-1] != b_:
                blk_seq.append(b_)
        blk_buf = {b_: i % 2 for i, b_ in enumerate(blk_seq)}
        loaded = set()

        def prefetch(hi):
            if hi >= len(heads):
                return
            kind, h = heads[hi]
            b_ = blk_of[hi]
            vv = VV[hi % 2]
            voff = {"A": 0, "B": 384, "C": 704}[kind] + h * 64
            P.dma("sp", vv[:, :, 0:64], V_v[:, :, voff:voff + 64], [VALL], [vv])
            if b_ in loaded:
                return
            loaded.add(b_)
            qt, kt_ = QT[blk_buf[b_]], KT[blk_buf[b_]]
            bi = b_[1]
            if kind == "A":
                P.dma("sp", qt[:, :], QKA[bi * 128:(bi + 1) * 128, :], [QKA], [qt])
                P.dma("sp", kt_[:, :], QKA[384 + bi * 128:384 + (bi + 1) * 128, :], [QKA], [kt_])
            elif kind == "B":
                nr = min(128, 320 - bi * 128)
                P.dma("sp", qt[0:nr, :], QKB[bi * 128:bi * 128 + nr, :], [QKB], [qt])
                P.dma("sp", kt_[0:nr, :], QKB[320 + bi * 128:320 + bi * 128 + nr, :], [QKB], [kt_])
            else:
                P.dma("sp", qt[0:96, :], QKC[bi, :, :], [QKC], [qt])
                P.dma("sp", kt_[0:96, :], QKC[5 + bi, :, :], [QKC], [kt_])

        def head(hi):
            kind, h = heads[hi]
            qt, kt_, vv = QT[blk_buf[blk_of[hi]]], KT[blk_buf[blk_of[hi]]], VV[hi % 2]
            prefetch(hi + 1)
            if kind == "A":
                dq, nmap, scale = 64, 1, 0.125
                rows = [(h % 2) * 64]
                zis = [4 + h % 2]
                orow, gcol = h * 64, hg[:, h:h + 1]
            elif kind == "B":
                dq, nmap, scale = 32, 2, 32 ** -0.5
                rows = [(h % 2) * 64, (h % 2) * 64 + 32]
                zis = [(h % 2) * 2, (h % 2) * 2 + 1]
                orow, gcol = 384 + h * 64, gB[:, 0:1]
            else:
                dq, nmap, scale = 96, 1, 96 ** -0.5
                rows, zis = [0], [None]
                orow, gcol = 704 + h * 64, hg[:, 6 + h:7 + h]

            def zcopy(qc):
                if kind == "C" or qc >= 8:
                    return
                for c in range(nmap):
                    z = Qz[zis[c]][qc % 2]
                    r0 = rows[c]
                    P.cp("pool", z[r0:r0 + dq, :], qt[r0:r0 + dq, qc * 512:(qc + 1) * 512], [qt], [z])

            units = []
            for qc in range(8):
                if kind == "A":
                    kts = [k for k in range(max(0, 4 * qc - 8), min(31, 4 * qc + 11) + 1)]
                else:
                    kts = list(range(NT))
                for ki, k in enumerate(kts):
                    for c in range(nmap):
                        units.append((qc, k, c, ki == 0, ki == len(kts) - 1))
            fin_q = []

            groups_ = []
            for qc in range(8):
                idxs = [i for i, u_ in enumerate(units) if u_[0] == qc]
                for g0 in range(0, len(idxs), 3):
                    groups_.append(idxs[g0:g0 + 3])

            def qk(p):
                sd = p % 2
                for u, i in enumerate(groups_[p]):
                    qc, k, c, first, last = units[i]
                    sb_ = psum[3 * sd + u]
                    if first and c == 0:
                        zcopy(qc + 1)
                    if kind == "C":
                        P.mm(sb_[:, :], kt_[0:96, k * 128:(k + 1) * 128], qt[0:96, qc * 512:(qc + 1) * 512], True, True, [kt_, qt], [sb_])
                    else:
                        z = Qz[zis[c]][qc % 2]
                        P.mm(sb_[:, :], kt_[:, k * 128:(k + 1) * 128], z[:, :], True, True, [kt_, z], [sb_])

            def ex(p):
                sd = p % 2
                g = groups_[p]
                n_ = len(g)
                qc, k, c, first, last = units[g[0]]
                bks = [psum[3 * sd + u] for u in range(n_)]
                P.act(Pt[p % 4][:, 0:n_ * 512], PSALL[:, sd * 1536:sd * 1536 + n_ * 512], AF.Exp, bks, [Pt[p % 4]], scale=scale)
                if kind == "A":
                    oi = (k * 128 - qc * 512 + 1024) // 128
                    P.tt("dve", Pm[p % 4][:, 0:n_ * 512], Pt[p % 4][:, 0:n_ * 512], amask[:, oi:oi + n_, :].rearrange("p a b -> p (a b)"), ALU.mult, [Pt[p % 4], amask], [Pm[p % 4]])

            def pv(p):
                for u, i in enumerate(groups_[p]):
                    qc, k, c, first, last = units[i]
                    ob = psum[6 + c] if kind == "B" else psum[6 + qc % 2]
                    src = Pm[p % 4] if kind == "A" else Pt[p % 4]
                    P.mm(ob[:, :], vv[:, k, :], src[:, u * 512:(u + 1) * 512], first, last, [vv, src], [ob])
                    if last and c == nmap - 1:
                        fin_q.append([qc, p + 3])
                        if kind == "B":
                            P.cp("dve", fe[qc % 2][0][:], psum[6][:, :], [psum[6]], [fe[qc % 2][0]])
                            P.cp("dve", fe[qc % 2][1][:], psum[7][:, :], [psum[7]], [fe[qc % 2][1]])

            def finalize1(qc):
                f = fin[qc % 2]
                if kind == "B":
                    o1, o2 = fe[qc % 2][0], fe[qc % 2][1]
                    P.op("dve", lambda e: e.reciprocal(out=f[0][0:64, :], in_=o1[64:128, :]), r=[o1], w=[f[0]])
                    P.op("dve", lambda e: e.reciprocal(out=f[1][0:64, :], in_=o2[64:128, :]), r=[o2], w=[f[1]])
                    P.tt("dve", f[2][0:64, :], o1[0:64, :], f[0][0:64, :], ALU.mult, [o1, f[0]], [f[2]])
                    P.stt(f[3][0:64, :], o2[0:64, :], neglam, f[1][0:64, :], ALU.mult, ALU.mult, [o2, f[1], lsm], [f[3]])
                    P.tt("pool", f[2][0:64, :], f[2][0:64, :], f[3][0:64, :], ALU.add, [f[2], f[3]], [f[2]])
                    P.tt("pool", f[4][0:64, :], f[2][0:64, :], f[2][0:64, :], ALU.mult, [f[2]], [f[4]])
                else:
                    o1 = psum[6 + qc % 2]
                    P.cp("dve", f[2][:], o1[:, :], [o1], [f[2]])
                    P.tt("pool", f[4][:], f[2][:], f[2][:], ALU.mult, [f[2]], [f[4]])

            def finalize2(qc):
                f = fin[qc % 2]
                y = yb[qc % 2]
                rb = psum[RB_]
                if kind == "B":
                    P.mm(rb[0:64, :], m128[0:64, :], f[4][0:64, :], True, True, [m128, f[4]], [rb])
                    P.act(f[0][0:64, :], rb[0:64, :], AF.Ln, [rb], [f[0]], bias=1e-5, scale=1.0)
                else:
                    P.mm(rb[0:64, :], m128[:, :], f[4][:], True, True, [m128, f[4]], [rb])
                    P.act(f[0][0:64, :], rb[0:64, :], AF.Ln, [rb], [f[0]])
                d_ = f[2]
                P.act(f[1][0:64, :], f[0][0:64, :], AF.Exp, [f[0]], [f[1]], scale=-0.5)
                P.stt(y[:], d_[0:64, :], gcol, f[1][0:64, :], ALU.mult, ALU.mult, [d_, f[1], hg, gB], [y])
                P.dma("sp", OT[orow:orow + 64, qc * 512:(qc + 1) * 512], y[:], [y], [OT])

            fin2_q = []

            def service(i):
                while fin_q and fin_q[0][1] <= i:
                    qc_ = fin_q.pop(0)[0]
                    finalize1(qc_)
                    fin2_q.append([qc_, i + (10 if kind == "B" else 4)])
                while fin2_q and fin2_q[0][1] <= i:
                    finalize2(fin2_q.pop(0)[0])

            n = len(groups_)
            zcopy(0)
            for i in range(n + 2):
                if i < n:
                    qk(i)
                if 0 <= i - 2 < n:
                    pv(i - 2)
                if i < n:
                    ex(i)
                service(i)
            while fin_q:
                qc_ = fin_q.pop(0)[0]
                finalize1(qc_)
                fin2_q.append([qc_, 0])
            while fin2_q:
                finalize2(fin2_q.pop(0)[0])

        prefetch(0)
        for hi in range(len(heads)):
            head(hi)

    def phase3(l, x_src):
        wout_bf = P.sb([128, 8, D], BF16)
        for c in range(8):
            P.dma("pool", wout_bf[:, c, :], wout_in[l, :, c, :], [wout_in], [wout_bf])
        g_ffn = P.sb([128, D], F32)
        P.dma("sp", g_ffn[:], grow_in[l, 1:2, :].partition_broadcast(128), [grow_in], [g_ffn])
        wr = P.sb([128, 8, NE], F32)
        P.dma("sp", wr[:], wr_in[l, :, :, :], [wr_in], [wr])
        aff = P.sb([128, NT, NE], F32)

        def mk(shape, dt):
            return [P.sb(shape, dt) for _ in range(2)]
        otg = mk([128, 8, 512], BF16)
        xt = [P.sb([128, D], F32) for _ in range(4)]; xm = mk([128, D], F32); junk = mk([128, D], BF16)
        hf = mk([128, D], F32); hb = mk([128, D], BF16); hT = mk([128, D], F32)
        st = mk([128, 8], F32); lg = mk([128, NE], F32)
        OT_v = OT.ap().rearrange("(c p) t -> p c t", p=128)
        x_rows = x_src.ap().rearrange("(j p) d -> j p d", p=128)
        XR_rows = XR.ap().rearrange("(j p) d -> j p d", p=128)
        H_rows = H.ap().rearrange("(j p) d -> j p d", p=128)
        st2 = mk([128, 8], F32)

        def p3_load(j):
            if j >= NT:
                return
            if j % 4 == 0:
                g4_ = (j // 4) % 2
                P.dma("sp", otg[g4_][:], OT_v[:, :, j * 128:j * 128 + 512], [OT], [otg[g4_]])
            P.dma("sp", xt[j % 4][:], x_rows[j], [x_src], [xt[j % 4]])

        def p3a(j):
            pa = j % 2
            g4 = (j // 4) % 2
            tq = (j % 4) * 128
            for half in range(2):
                bk = psum[half]
                for c in range(8):
                    P.mm(bk[:, :], otg[g4][:, c, tq:tq + 128], wout_bf[:, c, half * 512:(half + 1) * 512], c == 0, c == 7, [otg[g4], wout_bf], [bk])
                P.tt("dve", xm[pa][:, half * 512:(half + 1) * 512], bk[:, :], xt[j % 4][:, half * 512:(half + 1) * 512], ALU.add, [bk, xt[j % 4]], [xm[pa]])
            P.dma("sp", XR_rows[j], xm[pa][:], [xm[pa]], [XR])
            s_ = st[pa]
            P.act(junk[pa][:], xm[pa][:], AF.Square, [xm[pa]], [junk[pa], s_], accum_out=s_[:, 0:1])
            P.act(s_[:, 1:2], s_[:, 0:1], AF.Ln, [s_], [s_], scale=1.0 / D, bias=1e-6)
            P.act(s_[:, 2:3], s_[:, 1:2], AF.Exp, [s_], [s_], scale=-0.5)
            P.stt(hf[pa][:], xm[pa][:], s_[:, 2:3], g_ffn[:], ALU.mult, ALU.mult, [xm[pa], s_, g_ffn], [hf[pa]])
            P.cp("pool", hb[pa][:], hf[pa][:], [hf[pa]], [hb[pa]])
            P.dma("sp", H_rows[j], hb[pa][:], [hb[pa]], [H])

        def p3b(j):
            pa = j % 2
            s_ = st2[pa]
            for c in range(8):
                bk = psum[2 + c // 4]
                P.tr(bk[:, (c % 4) * 128:(c % 4 + 1) * 128], hf[pa][:, c * 128:(c + 1) * 128], identf[:], [hf[pa], identf], [bk])
            P.cp("act", hT[pa][:, 0:512], psum[2][:, :], [psum[2]], [hT[pa]])
            P.cp("dve", hT[pa][:, 512:1024], psum[3][:, :], [psum[3]], [hT[pa]])
            lb = psum[4 + pa]
            for c in range(8):
                P.mm(lb[:, 0:NE], hT[pa][:, c * 128:(c + 1) * 128], wr[:, c, :], c == 0, c == 7, [hT[pa], wr], [lb])
            P.act(lg[pa][:], lb[:, 0:NE], AF.Exp, [lb], [lg[pa], s_], accum_out=s_[:, 3:4])
            P.op("dve", lambda e, a=s_[:, 4:5], b=s_[:, 3:4]: e.reciprocal(out=a, in_=b), r=[s_], w=[s_])
            P.ts("dve", aff[:, j, :], lg[pa][:], s_[:, 4:5], None, ALU.mult, ALU.bypass, [lg[pa], s_], [aff])

        p3_load(0)
        p3_load(1)
        for step in range(NT + 1):
            p3_load(step + 2)
            if step < NT:
                p3a(step)
            if 0 <= step - 1 < NT:
                p3b(step - 1)
        P.dma("sp", AFF[:, :], aff[:].rearrange("p j e -> p (j e)"), [aff], [AFF])

    def phase4(l):
        aff = P.sb([128, NT, NE], F32)
        P.dma("sp", aff[:].rearrange("p j e -> p (j e)"), AFF[:, :], [AFF], [aff])
        aff8 = P.sb([128, 4, 128], F32)
        for t in range(4):
            bk = psum[t % 2]
            P.tr(bk[:, 0:128], aff[:, 8 * t:8 * t + 8, :].rearrange("p j e -> p (j e)"), identf[:], [aff, identf], [bk])
            P.cp("act" if t % 2 else "dve", aff8[:, t, :], bk[:, 0:128], [bk], [aff8])
        gsum = P.sb([128, 128], F32)
        P.dma("sp", gsum[:], gsum_in[:, :], [gsum_in], [gsum])
        lo = P.sb([128, 1], F32); mid = P.sb([128, 1], F32); cnt = P.sb([128, 2], F32); sel = P.sb([128, 1], F32)
        junk = P.sb([128, 512], BF16)
        P.memset("dve", lo[:], 0.0, [lo])
        P.memset("dve", cnt[:], 0.0, [cnt])
        a8 = aff8[:].rearrange("p t q -> p (t q)")
        for it in range(30):
            w_ = 0.5 ** (it + 1)
            P.ts("dve", mid[:], lo[:], w_, None, ALU.add, ALU.bypass, [lo], [mid])
            P.ts("dve", junk[:], a8, mid[:, 0:1], 0.0, ALU.is_ge, ALU.add, [aff8, mid], [junk, cnt], accum_out=cnt[:, 0:1])
            P.mm(psum[2][:, 0:2], gsum[:], cnt[:, 0:2], True, True, [gsum, cnt], [psum[2]])
            P.ts("dve", sel[:], psum[2][:, 0:1], float(CAP) - 0.5, w_, ALU.is_ge, ALU.mult, [psum[2]], [sel])
            P.tt("dve", lo[:], lo[:], sel[:], ALU.add, [lo, sel], [lo])
        P.dma("sp", THR[:, :], lo[0:NE, :], [lo], [THR])
        thr = P.sb([128, NE], F32)
        P.dma("sp", thr[:], THR.ap().rearrange("e o -> o e").partition_broadcast(128), [THR], [thr])
        mk_ = P.sb([128, NT, NE], F32)
        mkb = P.sb([128, NT, NE], BF16)
        P.tt("dve", mk_[:], aff[:], bc(thr[:].unsqueeze(1), [128, NT, NE]), ALU.is_ge, [aff, thr], [mk_])
        P.cp("dve", mkb[:], mk_[:], [mk_], [mkb])
        tri = P.sb([128, 128], BF16); onesb = P.sb([128, 128], BF16)
        P.dma("sp", tri[:], tri_in[:, :], [tri_in], [tri])
        P.memset("dve", onesb[:], 1.0, [onesb])
        mkb2 = mkb[:].rearrange("p j e -> p (j e)")
        P.mm(psum[2][:, :], tri[:], mkb2, True, True, [tri, mkb], [psum[2]])
        P.mm(psum[3][:, :], onesb[:], mkb2, True, True, [onesb, mkb], [psum[3]])
        cntt = P.sb([128, NT, NE], F32); carry = P.sb([128, NT, NE], F32); posm = P.sb([128, NT, NE], F32)
        P.cp("act", cntt[:].rearrange("p j e -> p (j e)"), psum[3][:, :], [psum[3]], [cntt])
        P.memset("dve", carry[:, 0, :], 0.0, [carry])
        for j in range(1, NT):
            P.tt("dve", carry[:, j, :], carry[:, j - 1, :], cntt[:, j - 1, :], ALU.add, [carry, cntt], [carry])
        P.tt("dve", posm[:].rearrange("p j e -> p (j e)"), psum[2][:, :], carry[:].rearrange("p j e -> p (j e)"), ALU.add, [psum[2], carry], [posm])
        P.tt("dve", posm[:], posm[:], mk_[:], ALU.mult, [posm, mk_], [posm])
        P.ts("dve", posm[:], posm[:], -1.0, None, ALU.add, ALU.bypass, [posm], [posm])
        rhsE = P.sb([128, NT, NE, 6], BF16)
        P.memset("dve", rhsE[:], 0.0, [rhsE])
        tokab = P.sb([128, NT, 2], BF16)
        P.dma("sp", tokab[:], tokab_in[:, :, :], [tokab_in], [tokab])
        P.cp("dve", rhsE[:, :, :, 0:2], bc(tokab[:].unsqueeze(2), [128, NT, NE, 2]), [tokab], [rhsE])
        r1 = P.sb([128, NT, NE], F32); gp = P.sb([128, NT, NE], BF16); gpf = P.sb([128, NT, NE], F32)
        P.cp("dve", r1[:], aff[:], [aff], [r1])
        for k in range(3):
            P.cp("dve", gp[:], r1[:], [r1], [gp])
            P.cp("dve", rhsE[:, :, :, 2 + k:3 + k], gp[:].unsqueeze(3), [gp], [rhsE])
            if k < 2:
                P.cp("dve", gpf[:], gp[:], [gp], [gpf])
                P.tt("dve", r1[:], r1[:], gpf[:], ALU.subtract, [r1, gpf], [r1])
        iota = P.sb([128, CAP], mybir.dt.float16)
        P.dma("sp", iota[:], iota_in[:, :], [iota_in], [iota])
        OH = [P.sb([128, CAP], BF16) for _ in range(4)]
        RT = P.sb([6, NE, CAP], F32)
        k = 0
        for e in range(NE):
            bk = psum[4 + e % 2]
            for j in range(NT):
                oh = OH[k % 4]
                P.ts("dve", oh[:], iota[:], posm[:, j, e:e + 1], None, ALU.is_equal, ALU.bypass, [iota, posm], [oh])
                P.mm(bk[0:6, :], rhsE[:, j, e, :], oh[:], j == 0, j == NT - 1, [rhsE, oh], [bk])
                k += 1
            P.cp("act", RT[:, e, :], bk[0:6, :], [bk], [RT])
        IDXG = P.sb([128, 64, 6], F32)
        for e in range(NE):
            for c in range(4):
                i = e * 4 + c
                P.tr(psum[6][:, i * 6:(i + 1) * 6], RT[:, e, c * 128:(c + 1) * 128], identf[0:6, 0:6], [RT, identf], [psum[6]])
        P.cp("dve", IDXG[:].rearrange("p i k -> p (i k)"), psum[6][:, 0:384], [psum[6]], [IDXG])
        ig = P.sb([128, 64, 2], F32)
        P.stt(ig[:, :, 0:1], IDXG[:, :, 0:1], 64.0, IDXG[:, :, 1:2], ALU.mult, ALU.add, [IDXG], [ig])
        P.tt("dve", ig[:, :, 1:2], IDXG[:, :, 2:3], IDXG[:, :, 3:4], ALU.add, [IDXG], [ig])
        P.tt("dve", ig[:, :, 1:2], ig[:, :, 1:2], IDXG[:, :, 4:5], ALU.add, [IDXG, ig], [ig])
        P.dma("sp", IDXD[:, :], ig[:].rearrange("p i k -> p (i k)"), [ig], [IDXD])

    def phase5(l):
        ig = P.sb([128, 64, 2], F32)
        P.dma("sp", ig[:].rearrange("p i k -> p (i k)"), IDXD[:, :], [IDXD], [ig])
        idx = P.sb([128, 64], I32)
        gate = P.sb([128, 64], F32)
        P.cp("dve", idx[:], ig[:, :, 0], [ig], [idx])
        P.cp("dve", gate[:], ig[:, :, 1], [ig], [gate])
        G = [[P.sb([128, D], BF16) for _ in range(4)] for _ in range(2)]
        xeT = [P.sb([128, 8, CAP], BF16) for _ in range(2)]
        gT = [P.sb([128, NF, CAP], BF16) for _ in range(2)]
        WGb = [P.sb([128, 2, D], BF16) for _ in range(4)]
        WUb = [P.sb([128, 2, D], BF16) for _ in range(4)]
        WDb = [P.sb([128, NF, D], BF16) for _ in range(2)]
        sa = [P.sb([128, CAP], F32) for _ in range(2)]
        yo = [P.sb([128, D], F32) for _ in range(4)]
        groups = [(0, 2), (2, 2), (4, 2), (6, 2), (8, 2), (10, 1)]
        glist = [(e, gi) for e in range(NE) for gi in range(6)]

        def load_group(n):
            if n >= len(glist):
                return
            e, gi = glist[n]
            f0, nf = groups[gi]
            b = n % 4
            P.dma("pool", WGb[b][:, 0:nf, :], wg_in[l, e, f0:f0 + nf, :, :].rearrange("f p d -> p f d"), [wg_in], [WGb[b]])
            P.dma("pool", WUb[b][:, 0:nf, :], wu_in[l, e, f0:f0 + nf, :, :].rearrange("f p d -> p f d"), [wu_in], [WUb[b]])

        def load_wd(e):
            if e >= NE:
                return
            for f0, nf in ((0, 4), (4, 4), (8, 3)):
                P.dma("pool", WDb[e % 2][:, f0:f0 + nf, :], wd_in[l, e, f0 * 128:(f0 + nf) * 128, :].rearrange("(f p) d -> p f d", p=128), [wd_in], [WDb[e % 2]])

        def gathers(e):
            if e >= NE:
                return
            for c in range(4):
                i = e * 4 + c
                P.op("pool", lambda en, o=G[e % 2][c], ix=idx[:, i:i + 1]: en.indirect_dma_start(
                    out=o[:], out_offset=None, in_=H[:, :], in_offset=bass.IndirectOffsetOnAxis(ap=ix, axis=0)),
                    r=[idx, H], w=[G[e % 2][c]], dma=True)

        def pass2_block(e, c, half):
            eb = e % 2
            i = e * 4 + c
            bk = psum[6 + half]
            for f in range(NF):
                P.mm(bk[:, :], gT[eb][:, f, c * 128:(c + 1) * 128], WDb[eb][:, f, half * 512:(half + 1) * 512], f == 0, f == NF - 1, [gT[eb], WDb[eb]], [bk])
            if half == 0:
                P.ts("dve", yo[c][:, 0:512], bk[:, :], gate[:, i:i + 1], None, ALU.mult, ALU.bypass, [bk, gate], [yo[c]])
            else:
                P.act(yo[c][:, 512:1024], bk[:, :], AF.Copy, [bk, gate], [yo[c]], scale=gate[:, i:i + 1])
                P.op("pool", lambda en, o=yo[c], ix=idx[:, i:i + 1]: en.indirect_dma_start(
                    out=XR[:, :], out_offset=bass.IndirectOffsetOnAxis(ap=ix, axis=0), in_=o[:], in_offset=None, compute_op=ALU.add),
                    r=[idx, yo[c], XR], w=[XR], dma=True)

        gathers(0)
        load_group(0)
        load_group(1)
        load_group(2)
        load_wd(0)
        n = 0
        k = 0
        pend = []
        for e in range(NE):
            eb = e % 2
            gathers(e + 1)
            for c in range(4):
                tb = pbf(c % 2)
                for dc in range(8):
                    P.tr(tb[:, dc * 128:(dc + 1) * 128], G[eb][c][:, dc * 128:(dc + 1) * 128], identb[:], [G[eb][c], identb], [psum[c % 2]])
                P.cp("dve" if c % 2 else "act", xeT[eb][:, :, c * 128:(c + 1) * 128], tb[:, 0:D].rearrange("p (d t) -> p d t", t=128), [psum[c % 2]], [xeT[eb]])
            for gi in range(6):
                load_group(n + 3)
                f0, nf = groups[gi]
                b = n % 4
                n += 1
                for fi in range(nf):
                    f = f0 + fi
                    fb = k % 2
                    k += 1
                    pa_, pu_ = psum[2 + fb], psum[4 + fb]
                    for dc in range(8):
                        P.mm(pa_[:, :], WGb[b][:, fi, dc * 128:(dc + 1) * 128], xeT[eb][:, dc, :], dc == 0, dc == 7, [WGb[b], xeT[eb]], [pa_])
                    for dc in range(8):
                        P.mm(pu_[:, :], WUb[b][:, fi, dc * 128:(dc + 1) * 128], xeT[eb][:, dc, :], dc == 0, dc == 7, [WUb[b], xeT[eb]], [pu_])
                    P.act(sa[fb][:], pa_[:, :], AF.Silu, [pa_], [sa[fb]])
                    P.tt("dve", gT[eb][:, f, :], sa[fb][:], pu_[:, :], ALU.mult, [sa[fb], pu_], [gT[eb]])
                for _ in range(2):
                    if pend:
                        pass2_block(*pend.pop(0))
            while pend:
                pass2_block(*pend.pop(0))
            load_wd(e + 1)
            pend = [(e, c, half) for c in range(4) for half in range(2)]
        while pend:
            pass2_block(*pend.pop(0))

    def phase_final():
        g_f = P.sb([128, D], F32)
        P.dma("sp", g_f[:], fing_in[0:1, :].partition_broadcast(128), [fing_in], [g_f])
        NB = 6
        xt = [P.sb([128, D], F32) for _ in range(NB)]
        junk = [P.sb([128, D], BF16) for _ in range(2)]
        yo = [P.sb([128, D], F32) for _ in range(4)]
        st = [P.sb([128, 4], F32) for _ in range(4)]
        XR_rows = XR.ap().rearrange("(j p) d -> j p d", p=128)
        O_rows = out_d.ap().rearrange("(j p) d -> j p d", p=128)
        for j in range(min(NB - 1, NT)):
            P.dma("sp", xt[j % NB][:], XR_rows[j], [XR], [xt[j % NB]])
        for j in range(NT):
            if j + NB - 1 < NT:
                jj = j + NB - 1
                P.dma("sp", xt[jj % NB][:], XR_rows[jj], [XR], [xt[jj % NB]])
            x_ = xt[j % NB]
            s_ = st[j % 4]
            y_ = yo[j % 4]
            P.act(junk[j % 2][:], x_[:], AF.Square, [x_], [junk[j % 2], s_], accum_out=s_[:, 0:1])
            P.act(s_[:, 1:2], s_[:, 0:1], AF.Ln, [s_], [s_], scale=1.0 / D, bias=1e-6)
            P.act(s_[:, 2:3], s_[:, 1:2], AF.Exp, [s_], [s_], scale=-0.5)
            P.stt(y_[:], x_[:], s_[:, 2:3], g_f[:], ALU.mult, ALU.mult, [x_, s_, g_f], [y_])
            P.dma("pool", O_rows[j], y_[:], [y_], [out_d])

    return P, dict(phase0=phase0, p1=phase1, p2=phase2, p3=phase3, p4=phase4, p5=phase5, final=phase_final,
                   x_in=x_in, XR=XR)


def build(n_layers=DEPTH, upto="all", debug=False):
    import contextlib
    with contextlib.ExitStack() as es_glob:
        return _build(es_glob, n_layers, upto, debug)


def _build(es_glob, n_layers, upto, debug):
    import contextlib
    P, ph = _define(es_glob, debug)
    nc = P.nc

    def run(fn, *a):
        with contextlib.ExitStack() as es:
            P.es = es
            fn(*a)
            P.barrier()
            P.emit()
        P.es = es_glob

    run(ph["phase0"])
    done = False
    for l in range(n_layers):
        x_src = ph["x_in"] if l == 0 else ph["XR"]
        for name in ["p1", "p2", "p3", "p4", "p5"]:
            if name in ("p1", "p3"):
                run(ph[name], l, x_src)
            else:
                run(ph[name], l)
            if upto != "all" and (l, name) == tuple(upto):
                done = True
                break
        if done:
            break
    if upto == "all":
        run(ph["final"])
    return nc


def _consts():
    c = {}
    c["identb"] = np.eye(128, dtype=np.float32).astype(ml_dtypes.bfloat16)
    c["identf"] = np.eye(128, dtype=np.float32)
    fa = 1.0 / (500000.0 ** (np.arange(0, 16, 2, dtype=np.float32) / 16))
    fb = 1.0 / (500000.0 ** (np.arange(0, 8, 2, dtype=np.float32) / 8))
    fc = 1.0 / (10000.0 ** (np.arange(0, 32, 2, dtype=np.float32) / 32))
    inv = np.concatenate([fa, fb, fc]).astype(np.float64) / (2 * np.pi)
    c["invf"] = np.tile(inv.astype(np.float32)[None, :], (128, 1))
    i = np.arange(128)[:, None]
    jq = np.arange(512)[None, :]
    am = np.zeros((128, 20, 512), np.float32)
    for oi in range(20):
        o = oi * 128 - 1024
        dl = o + i - jq
        a = np.abs(dl)
        am[:, oi, :] = (a <= 64).astype(np.float32) + ((dl % 4 == 0) & (a <= 256)) + ((dl % 16 == 0) & (a <= 1024))
    c["amask"] = am.astype(ml_dtypes.bfloat16)
    c["gsum"] = (np.arange(128)[:, None] % 16 == np.arange(128)[None, :] % 16).astype(np.float32)
    c["tri"] = (np.arange(128)[:, None] <= np.arange(128)[None, :]).astype(np.float32).astype(ml_dtypes.bfloat16)
    c["iota"] = np.tile(np.arange(512, dtype=np.float16)[None, :], (128, 1))
    t = np.arange(NT)[None, :] * 128 + np.arange(128)[:, None]
    c["tokab"] = np.stack([t // 64, t % 64], axis=-1).astype(np.float32).astype(ml_dtypes.bfloat16)
    return c


def prep_inputs(x, positions, attn_norm_g, w_in, lam_q1, lam_k1, lam_q2, lam_k2, diff_subln_g,
                mla_q_norm_g, mla_w_uq, mla_kv_norm_g, mla_w_ukv, dil_out_g, mla_out_g, w_out,
                ffn_norm_g, w_router, w_gate, w_up, w_down, final_norm_g):
    f = lambda a: np.ascontiguousarray(np.asarray(a, dtype=np.float32))
    sh = dict(_consts())
    sh["win"] = f(np.asarray(w_in).reshape(DEPTH, 8, 128, INW).transpose(0, 2, 1, 3))
    sh["wout"] = f(np.asarray(w_out).reshape(DEPTH, 8, 128, D).transpose(0, 2, 1, 3))
    sh["wuq"] = f(mla_w_uq)
    sh["wukv"] = f(mla_w_ukv)
    sh["wr"] = f(np.asarray(w_router).reshape(DEPTH, 8, 128, NE).transpose(0, 2, 1, 3))
    sh["wg"] = f(np.asarray(w_gate).reshape(DEPTH, NE, 8, 128, NF, 128).transpose(0, 1, 4, 3, 2, 5).reshape(DEPTH, NE, NF, 128, D))
    sh["wu"] = f(np.asarray(w_up).reshape(DEPTH, NE, 8, 128, NF, 128).transpose(0, 1, 4, 3, 2, 5).reshape(DEPTH, NE, NF, 128, D))
    sh["wd"] = f(w_down)
    sh["grow"] = f(np.stack([np.asarray(attn_norm_g), np.asarray(ffn_norm_g)], axis=1))
    sh["fing"] = f(np.asarray(final_norm_g).reshape(1, D))
    sh["qng"] = f(mla_q_norm_g)
    sh["kvng"] = f(mla_kv_norm_g)
    hg = np.zeros((DEPTH, 64, 12), np.float32)
    hg[:, :, 0:6] = np.asarray(dil_out_g).reshape(DEPTH, 6, 64).transpose(0, 2, 1)
    hg[:, :, 6:11] = np.asarray(mla_out_g).reshape(DEPTH, 5, 64).transpose(0, 2, 1)
    hg[:, :, 11] = np.asarray(diff_subln_g)
    sh["hg"] = hg
    sh["lam"] = f(np.stack([np.asarray(lam_q1), np.asarray(lam_k1), np.asarray(lam_q2), np.asarray(lam_k2)], axis=1))
    xs = np.asarray(x, dtype=np.float32)
    ps = np.asarray(positions).astype(np.int32)
    in_maps = []
    for b in range(8):
        m = dict(sh)
        m["x"] = np.ascontiguousarray(xs[b])
        m["pos"] = np.ascontiguousarray(ps[b].reshape(NT, 128).T)
        in_maps.append(m)
    return in_maps


_NC_CACHE = {}


def kernel(**inputs):
    in_maps = prep_inputs(**inputs)
    if "nc" not in _NC_CACHE:
        _NC_CACHE["nc"] = build()
    res = run_bass_kernel_spmd(_NC_CACHE["nc"], in_maps, core_ids=list(range(8)))
    return np.stack([np.asarray(r["out"], dtype=np.float32) for r in res.results], axis=0)
```

```python
import math
import numpy as np
import ml_dtypes
import concourse.bass as bass
import concourse.mybir as mybir
from concourse.bass_utils import run_bass_kernel_spmd

F32 = mybir.dt.float32
BF16 = mybir.dt.bfloat16
I32 = mybir.dt.int32
AF = mybir.ActivationFunctionType
ALU = mybir.AluOpType

S = 4096
D = 1024
NT = 32
DEPTH = 2
INW = 2464
NE = 16
CAP = 512
FF = 1408
NF = 11
NDMASEM = 48


class Unit:
    __slots__ = ("last_w", "rd_eng", "rd_dma")

    def __init__(self):
        self.last_w = None
        self.rd_eng = {}
        self.rd_dma = []


class Op:
    __slots__ = ("eng", "fn", "deps", "is_dma", "signal", "sem", "val", "epoch")

    def __init__(self, eng, fn, is_dma):
        self.eng = eng
        self.fn = fn
        self.deps = []
        self.is_dma = is_dma
        self.signal = False
        self.sem = None
        self.val = 0
        self.epoch = 0


class T:
    def __init__(self, h):
        self.h = h
        self.u = Unit()

    def __getitem__(self, k):
        return self.h[k]

    def ap(self):
        return self.h.ap()


class Bank:
    def __init__(self, ap):
        self.a = ap
        self.u = Unit()

    def __getitem__(self, k):
        return self.a[k]


class Prog:
    def __init__(self, nc):
        self.nc = nc
        self.ops = []
        self.engs = {"pe": nc.tensor, "act": nc.scalar, "dve": nc.vector, "pool": nc.gpsimd, "sp": nc.sync}
        self.uid = 0
        self.epoch = 0
        self.last_eng = {}
        self.open_dma = []
        self.es = None
        self.st = None

    def sb(self, shape, dtype):
        self.uid += 1
        return T(self.es.enter_context(self.nc.sbuf_tensor(f"sb{self.uid}", list(shape), dtype)))

    def ps_all(self):
        return self.nc.alloc_psum_tensor("psall", [128, 4096], F32)

    def dram(self, name, shape, dtype, kind="Internal"):
        return T(self.nc.dram_tensor(name, list(shape), dtype, kind=kind))

    def _dep(self, op, d):
        if d is None or d is op:
            return
        if not (op.is_dma or d.is_dma) and d.eng == "pe" and op.eng == "pe":
            return
        if d not in op.deps:
            op.deps.append(d)
            d.signal = True

    def op(self, eng, fn, r=(), w=(), dma=False):
        o = Op(eng, fn, dma)
        o.epoch = self.epoch
        us_r = [t.u if hasattr(t, 'u') else t for t in r]
        us_w = [t.u if hasattr(t, 'u') else t for t in w]
        for u in us_r:
            self._dep(o, u.last_w)
        for u in us_w:
            lw = u.last_w
            if lw is not None and (o.is_dma or lw.is_dma or lw.eng != o.eng):
                self._dep(o, lw)
            for e, rd in u.rd_eng.items():
                if o.is_dma or e != o.eng:
                    self._dep(o, rd)
            for rd in u.rd_dma:
                self._dep(o, rd)
        for u in us_r:
            if dma:
                u.rd_dma.append(o)
            else:
                u.rd_eng[eng] = o
        for u in us_w:
            u.last_w = o
            u.rd_eng = {}
            u.rd_dma = []
        self.ops.append(o)
        if dma:
            self.open_dma.append(o)
        else:
            self.last_eng[eng] = o
        return o

    def barrier(self):
        pend = list(self.last_eng.values()) + self.open_dma[-NDMASEM:]
        self.open_dma = self.open_dma[-NDMASEM:]
        news = []
        for eng in ("pe", "act", "dve", "pool", "sp"):
            o = Op(eng, lambda e: e.nop(), False)
            o.epoch = self.epoch
            for d in pend:
                if d.eng == eng and not d.is_dma:
                    continue
                o.deps.append(d)
                d.signal = True
            self.ops.append(o)
            news.append(o)
        self.epoch += 1
        self.last_eng = {}

    def dma(self, q, out, in_, r, w):
        return self.op(q, lambda e: e.dma_start(out=out, in_=in_), r=r, w=w, dma=True)

    def mm(self, out, lhsT, rhs, start, stop, r, w):
        return self.op("pe", lambda e: e.matmul(out, lhsT=lhsT, rhs=rhs, start=start, stop=stop), r=r, w=w)

    def tr(self, out, in_, ident, r, w):
        return self.op("pe", lambda e: e.transpose(out, in_, ident), r=r, w=w)

    def act(self, out, in_, func, r, w, **kw):
        return self.op("act", lambda e: e.activation(out=out, in_=in_, func=func, **kw), r=r, w=w)

    def cp(self, eng, out, in_, r, w):
        if eng == "act":
            return self.op("act", lambda e: e.copy(out=out, in_=in_), r=r, w=w)
        return self.op(eng, lambda e: e.tensor_copy(out=out, in_=in_), r=r, w=w)

    def tt(self, eng, out, in0, in1, op, r, w):
        return self.op(eng, lambda e: e.tensor_tensor(out=out, in0=in0, in1=in1, op=op), r=r, w=w)

    def ts(self, eng, out, in0, s1, s2, op0, op1, r, w, **kw):
        return self.op(eng, lambda e: e.tensor_scalar(out=out, in0=in0, scalar1=s1, scalar2=s2, op0=op0, op1=op1, **kw), r=r, w=w)

    def stt(self, out, in0, scalar, in1, op0, op1, r, w):
        return self.op("dve", lambda e: e.scalar_tensor_tensor(out=out, in0=in0, scalar=scalar, in1=in1, op0=op0, op1=op1), r=r, w=w)

    def memset(self, eng, ap, val, w):
        return self.op(eng, lambda e: e.memset(ap, val), w=w)

    def emit(self):
        nc = self.nc
        if self.st is None:
            self.st = dict(esems={}, dsem=[nc.alloc_semaphore(f"ds{i}") for i in range(NDMASEM)], cnt={},
                           seen={k: {} for k in self.engs}, ndma=0)
        st = self.st
        esems, dsem, cnt, seen, ndma = st["esems"], st["dsem"], st["cnt"], st["seen"], st["ndma"]
        ops, self.ops = self.ops, []
        for o in ops:
            e = self.engs[o.eng]
            sn = seen[o.eng]
            for d in o.deps:
                key = id(d.sem)
                if sn.get(key, 0) < d.val:
                    e.wait_ge(d.sem, d.val)
                    sn[key] = d.val
            if o.is_dma:
                s = dsem[ndma % NDMASEM]
                v = 16 * (ndma // NDMASEM + 1)
                ndma += 1
                key = id(s)
                if v > 16 and sn.get(key, 0) < v - 16:
                    e.wait_ge(s, v - 16)
                    sn[key] = v - 16
                ins = o.fn(e)
                ins.then_inc(s, 16)
                o.sem, o.val = s, v
            else:
                ins = o.fn(e)
                if o.signal:
                    k = (o.eng, o.epoch)
                    if k not in esems:
                        esems[k] = nc.alloc_semaphore(f"es_{o.eng}_{o.epoch}")
                        cnt[k] = 0
                    cnt[k] += 1
                    o.sem, o.val = esems[k], cnt[k]
                    ins.then_inc(o.sem, 1)
        st["ndma"] = ndma
        return ndma


def bc(ap, shape):
    return ap.broadcast_to(list(shape))


def _define(es_glob, debug):
    nc = bass.Bass("TRN2", target_bir_lowering=False)
    P = Prog(nc)
    P.es = es_glob
    kin = "ExternalInput"
    kdbg = "ExternalOutput" if debug else "Internal"
    x_in = P.dram("x", [S, D], F32, kin)
    pos_in = P.dram("pos", [128, NT], I32, kin)
    win_in = P.dram("win", [DEPTH, 128, 8, INW], F32, kin)
    wout_in = P.dram("wout", [DEPTH, 128, 8, D], F32, kin)
    wuq_in = P.dram("wuq", [DEPTH, 192, 480], F32, kin)
    wukv_in = P.dram("wukv", [DEPTH, 128, 640], F32, kin)
    wr_in = P.dram("wr", [DEPTH, 128, 8, NE], F32, kin)
    nle = 1 if debug == "noexp" else DEPTH
    nee = 1 if debug == "noexp" else NE
    wg_in = P.dram("wg", [nle, nee, NF, 128, D], F32, kin)
    wu_in = P.dram("wu", [nle, nee, NF, 128, D], F32, kin)
    wd_in = P.dram("wd", [nle, nee, FF, D], F32, kin)
    grow_in = P.dram("grow", [DEPTH, 2, D], F32, kin)
    fing_in = P.dram("fing", [1, D], F32, kin)
    qng_in = P.dram("qng", [DEPTH, 192], F32, kin)
    kvng_in = P.dram("kvng", [DEPTH, 128], F32, kin)
    hg_in = P.dram("hg", [DEPTH, 64, 12], F32, kin)
    lam_in = P.dram("lam", [DEPTH, 4, 32], F32, kin)
    identb_in = P.dram("identb", [128, 128], BF16, kin)
    identf_in = P.dram("identf", [128, 128], F32, kin)
    invf_in = P.dram("invf", [128, 28], F32, kin)
    amask_in = P.dram("amask", [128, 20, 512], BF16, kin)
    tri_in = P.dram("tri", [128, 128], BF16, kin)
    gsum_in = P.dram("gsum", [128, 128], F32, kin)
    iota_in = P.dram("iota", [128, 512], mybir.dt.float16, kin)
    tokab_in = P.dram("tokab", [128, NT, 2], BF16, kin)
    out_d = P.dram("out", [S, D], F32, "ExternalOutput")
    QKA = P.dram("QKA", [768, S], BF16, kdbg)
    QKB = P.dram("QKB", [640, S], BF16, kdbg)
    QKC = P.dram("QKC", [10, 96, S], BF16, kdbg)
    VALL = P.dram("VALL", [S, D], BF16, kdbg)
    OT = P.dram("OT", [D, S], BF16, kdbg)
    XR = P.dram("XR", [S, D], F32, kdbg)
    H = P.dram("H", [S, D], BF16, kdbg)
    THR = P.dram("THR", [NE, 1], F32, kdbg)
    AFF = P.dram("AFF", [128, NT * NE], F32, kdbg)
    IDXD = P.dram("IDXD", [128, 64 * 2], F32, kdbg)

    PSALL = P.ps_all()
    psum = [Bank(PSALL[:, i * 512:(i + 1) * 512]) for i in range(8)]

    def pbf(i):
        return psum[i].a.bitcast(BF16)

    identb = P.sb([128, 128], BF16)
    identf = P.sb([128, 128], F32)
    P.dma("sp", identb[:], identb_in[:, :], [identb_in], [identb])
    P.dma("sp", identf[:], identf_in[:, :], [identf_in], [identf])
    sin_all = P.sb([128, NT, 28], F32)
    cos_all = P.sb([128, NT, 28], F32)


    def phase0():
        invf = P.sb([128, 28], F32)
        posi = P.sb([128, NT], I32)
        posf = P.sb([128, NT], F32)
        tt_ = P.sb([128, NT, 28], F32)
        ki = P.sb([128, NT, 28], I32)
        kf = P.sb([128, NT, 28], F32)
        fr = P.sb([128, NT, 28], F32)
        P.dma("sp", invf[:], invf_in[:, :], [invf_in], [invf])
        P.dma("sp", posi[:], pos_in[:, :], [pos_in], [posi])
        P.cp("dve", posf[:], posi[:], [posi], [posf])
        P.tt("dve", tt_[:], bc(posf[:].unsqueeze(2), [128, NT, 28]), bc(invf[:].unsqueeze(1), [128, NT, 28]), ALU.mult, [posf, invf], [tt_])
        for dst, shift in ((sin_all, 0.0), (cos_all, 0.25)):
            if shift:
                P.ts("dve", tt_[:], tt_[:], shift, None, ALU.add, ALU.bypass, [tt_], [tt_])
            P.cp("dve", ki[:], tt_[:], [tt_], [ki])
            P.cp("dve", kf[:], ki[:], [ki], [kf])
            P.tt("dve", fr[:], tt_[:], kf[:], ALU.subtract, [tt_, kf], [fr])
            P.ts("dve", fr[:], fr[:], 0.5, -0.5, ALU.min, ALU.max, [fr], [fr])
            P.act(dst[:], fr[:], AF.Sin, [fr], [dst], scale=2.0 * math.pi)

    def phase1(l, x_src):
        win_bf = P.sb([128, 8, INW], BF16)
        for c in range(8):
            for h_ in range(2):
                P.dma("pool", win_bf[:, c, h_ * 1232:(h_ + 1) * 1232], win_in[l, :, c, h_ * 1232:(h_ + 1) * 1232], [win_in], [win_bf])
        wuq_f = P.sb([128, 2, 480], F32)
        wuq_bf = P.sb([128, 2, 480], BF16)
        wukv_f = P.sb([128, 640], F32)
        wukv_bf = P.sb([128, 640], BF16)
        P.dma("sp", wuq_f[:, 0, :], wuq_in[l, 0:128, :], [wuq_in], [wuq_f])
        P.dma("sp", wuq_f[0:64, 1, :], wuq_in[l, 128:192, :], [wuq_in], [wuq_f])
        P.dma("sp", wukv_f[:], wukv_in[l, :, :], [wukv_in], [wukv_f])
        P.cp("dve", wuq_bf[:, 0, :], wuq_f[:, 0, :], [wuq_f], [wuq_bf])
        P.cp("dve", wuq_bf[0:64, 1, :], wuq_f[0:64, 1, :], [wuq_f], [wuq_bf])
        P.cp("dve", wukv_bf[:], wukv_f[:], [wukv_f], [wukv_bf])
        g_attn = P.sb([128, D], F32)
        g_qn = P.sb([128, 192], F32)
        g_kvn = P.sb([128, 128], F32)
        P.dma("sp", g_attn[:], grow_in[l, 0:1, :].partition_broadcast(128), [grow_in], [g_attn])
        P.dma("sp", g_qn[:], qng_in[l:l + 1, :].partition_broadcast(128), [qng_in], [g_qn])
        P.dma("sp", g_kvn[:], kvng_in[l:l + 1, :].partition_broadcast(128), [kvng_in], [g_kvn])

        def mk(shape, dt):
            return [P.sb(shape, dt) for _ in range(2)]
        xt = [P.sb([128, D], F32) for _ in range(4)]; junk = mk([128, D], BF16); xn = mk([128, D], BF16); hT = mk([128, D], BF16)
        st1 = mk([128, 8], F32)
        st2 = mk([128, 8], F32)
        pr = mk([128, INW], F32)
        qkA = mk([128, 768], BF16); qkB = mk([128, 640], BF16); vst = mk([128, D], BF16)
        rt = mk([128, 4, 160], F32)
        cqn = mk([128, 192], BF16); ckvn = mk([128, 128], BF16); cT = mk([128, 384], BF16)
        qcs = mk([128, 480], F32); kvs = mk([128, 640], F32)
        QCb = mk([128, 5, 96], BF16); KCb = mk([128, 5, 96], BF16); kr = mk([128, 32], F32)
        stA = mk([128, 768], BF16); stB = mk([128, 640], BF16); stC = mk([128, 10, 128], BF16)
        x_rows = x_src.ap().rearrange("(j p) d -> j p d", p=128)
        QKA_v = QKA.ap().rearrange("(c p) t -> p c t", p=128)
        QKB_v = QKB.ap().rearrange("(c p) t -> p c t", p=128)
        QKC_v = QKC.ap().rearrange("i r t -> r i t")
        V_rows = VALL.ap().rearrange("(j p) d -> j p d", p=128)
        T0, T1, PJ0, PJ1, PQ, PK0, PK1 = 0, 1, 2, 3, 4, 5, 6

        def rms(src_ap, n, eps, stt_, c0, r, pa):
            P.act(junk[pa][:, 0:n], src_ap, AF.Square, r, [junk[pa], stt_], accum_out=stt_[:, c0:c0 + 1])
            P.act(stt_[:, c0 + 1:c0 + 2], stt_[:, c0:c0 + 1], AF.Ln, [stt_], [stt_], scale=1.0 / n, bias=eps)
            P.act(stt_[:, c0 + 2:c0 + 3], stt_[:, c0 + 1:c0 + 2], AF.Exp, [stt_], [stt_], scale=-0.5)

        def rope(src3, dst3, lo, half, fo, nf, j, pa, rd, wr_, tmp):
            G = src3.shape[1]
            x1 = src3[:, :, lo:lo + half]; x2 = src3[:, :, lo + half:lo + 2 * half]
            cs = bc(cos_all[:, j, fo:fo + nf].unsqueeze(1), [128, G, half])
            sn = bc(sin_all[:, j, fo:fo + nf].unsqueeze(1), [128, G, half])
            n = G * half
            ta = tmp[:, 0, 0:n].rearrange("p (g k) -> p g k", k=half)
            tb = tmp[:, 1, 0:n].rearrange("p (g k) -> p g k", k=half)
            tc = tmp[:, 2, 0:n].rearrange("p (g k) -> p g k", k=half)
            td = tmp[:, 3, 0:n].rearrange("p (g k) -> p g k", k=half)
            P.tt("dve", ta, x1, cs, ALU.mult, rd + [cos_all], [tmp])
            P.tt("dve", tb, x2, sn, ALU.mult, rd + [sin_all], [tmp])
            P.tt("dve", tc, x2, cs, ALU.mult, rd + [cos_all], [tmp])
            P.tt("dve", td, x1, sn, ALU.mult, rd + [sin_all], [tmp])
            P.tt("dve", dst3[:, :, lo:lo + half], ta, tb, ALU.subtract, [tmp], wr_)
            P.tt("dve", dst3[:, :, lo + half:lo + 2 * half], tc, td, ALU.add, [tmp], wr_)

        def load_x(j):
            if j < NT:
                P.dma("sp", xt[j % 4][:], x_rows[j], [x_src], [xt[j % 4]])

        def s0(j):
            pa = j % 2
            x4 = xt[j % 4]
            rms(x4[:], D, 1e-6, st1[pa], 0, [x4], pa)
            yield
            P.stt(xn[pa][:], x4[:], st1[pa][:, 2:3], g_attn[:], ALU.mult, ALU.mult, [x4, st1[pa], g_attn], [xn[pa]])
            yield

        def s1f(j):
            pa = j % 2
            tb = pbf(T0)
            for c in range(8):
                P.tr(tb[:, c * 128:(c + 1) * 128], xn[pa][:, c * 128:(c + 1) * 128], identb[:], [xn[pa], identb], [psum[T0]])
            P.cp("act", hT[pa][:, 0:512], tb[:, 0:512], [psum[T0]], [hT[pa]])
            P.cp("dve", hT[pa][:, 512:1024], tb[:, 512:1024], [psum[T0]], [hT[pa]])

        def s1b(j, feed):
            pa = j % 2
            col = 0
            for cc in range(5):
                n = min(512, INW - col)
                bk = (PJ0, PJ1, 7)[cc % 3]
                for c in range(8):
                    P.mm(psum[bk][:, 0:n], hT[pa][:, c * 128:(c + 1) * 128], win_bf[:, c, col:col + n], c == 0, c == 7, [hT[pa], win_bf], [psum[bk]])
                feed(4)
                P.cp("act" if cc % 2 == 0 else "dve", pr[pa][:, col:col + n], psum[bk][:, 0:n], [psum[bk]], [pr[pa]])
                col += n

        def s2a(j):
            pa = j % 2
            p_ = pr[pa]
            P.cp("act", qkA[pa][:], p_[:, 0:768], [p_], [qkA[pa]])
            yield
            P.cp("pool", qkB[pa][:], p_[:, 1152:1792], [p_], [qkB[pa]])
            yield
            rope(p_[:, 0:768].rearrange("p (g k) -> p g k", k=64), qkA[pa][:].rearrange("p (g k) -> p g k", k=64), 0, 8, 0, 8, j, pa, [p_], [qkA[pa]], rt[pa])
            yield
            rope(p_[:, 1152:1792].rearrange("p (g k) -> p g k", k=32), qkB[pa][:].rearrange("p (g k) -> p g k", k=32), 0, 4, 8, 4, j, pa, [p_], [qkB[pa]], rt[pa])
            yield
            P.cp("pool", vst[pa][:, 0:384], p_[:, 768:1152], [p_], [vst[pa]])
            yield
            P.cp("pool", vst[pa][:, 384:704], p_[:, 1792:2112], [p_], [vst[pa]])
            yield
            rms(p_[:, 2112:2304], 192, 1e-6, st2[pa], 0, [p_], pa)
            yield
            P.stt(cqn[pa][:], p_[:, 2112:2304], st2[pa][:, 2:3], g_qn[:], ALU.mult, ALU.mult, [p_, st2[pa], g_qn], [cqn[pa]])
            yield
            rms(p_[:, 2304:2432], 128, 1e-6, st2[pa], 3, [p_], pa)
            yield
            P.stt(ckvn[pa][:], p_[:, 2304:2432], st2[pa][:, 5:6], g_kvn[:], ALU.mult, ALU.mult, [p_, st2[pa], g_kvn], [ckvn[pa]])
            yield
            rope(p_[:, 2432:2464].unsqueeze(1), kr[pa][:].unsqueeze(1), 0, 16, 12, 16, j, pa, [p_], [kr[pa]], rt[pa])
            yield

        def s2b(j):
            pa = j % 2
            tA = pbf(T1)
            for c in range(6):
                P.tr(tA[:, c * 128:(c + 1) * 128], qkA[pa][:, c * 128:(c + 1) * 128], identb[:], [qkA[pa], identb], [psum[T1]])
            P.cp("act", stA[pa][:], tA[:, 0:768], [psum[T1]], [stA[pa]])
            P.dma("sp", QKA_v[:, :, j * 128:(j + 1) * 128], stA[pa][:].rearrange("p (c t) -> p c t", t=128), [stA[pa]], [QKA])
            tB = pbf(T0)
            for c in range(5):
                P.tr(tB[:, c * 128:(c + 1) * 128], qkB[pa][:, c * 128:(c + 1) * 128], identb[:], [qkB[pa], identb], [psum[T0]])
            P.cp("dve", stB[pa][:], tB[:, 0:640], [psum[T0]], [stB[pa]])
            P.dma("sp", QKB_v[:, :, j * 128:(j + 1) * 128], stB[pa][:].rearrange("p (c t) -> p c t", t=128), [stB[pa]], [QKB])
            tM = pbf(T1)
            P.tr(tM[:, 0:128], cqn[pa][:, 0:128], identb[:], [cqn[pa], identb], [psum[T1]])
            P.tr(tM[0:64, 128:256], cqn[pa][:, 128:192], identb[:], [cqn[pa], identb], [psum[T1]])
            P.tr(tM[:, 256:384], ckvn[pa][:], identb[:], [ckvn[pa], identb], [psum[T1]])
            P.cp("act", cT[pa][:, 0:128], tM[:, 0:128], [psum[T1]], [cT[pa]])
            P.cp("act", cT[pa][0:64, 128:256], tM[0:64, 128:256], [psum[T1]], [cT[pa]])
            P.cp("act", cT[pa][:, 256:384], tM[:, 256:384], [psum[T1]], [cT[pa]])
            P.mm(psum[PQ][:, 0:480], cT[pa][:, 0:128], wuq_bf[:, 0, :], True, False, [cT[pa], wuq_bf], [psum[PQ]])
            P.mm(psum[PQ][:, 0:480], cT[pa][0:64, 128:256], wuq_bf[0:64, 1, :], False, True, [cT[pa], wuq_bf], [psum[PQ]])
            P.mm(psum[PK0][:, 0:320], cT[pa][:, 256:384], wukv_bf[:, 0:320], True, True, [cT[pa], wukv_bf], [psum[PK0]])
            P.mm(psum[PK1][:, 0:320], cT[pa][:, 256:384], wukv_bf[:, 320:640], True, True, [cT[pa], wukv_bf], [psum[PK1]])
            P.cp("dve", qcs[pa][:], psum[PQ][:, 0:480], [psum[PQ]], [qcs[pa]])
            P.cp("act", kvs[pa][:, 0:320], psum[PK0][:, 0:320], [psum[PK0]], [kvs[pa]])
            P.cp("dve", kvs[pa][:, 320:640], psum[PK1][:, 0:320], [psum[PK1]], [kvs[pa]])

        def s2c_pre(j):
            pa = j % 2
            q3 = qcs[pa][:].rearrange("p (g k) -> p g k", k=96)
            P.cp("pool", QCb[pa][:], q3, [qcs[pa]], [QCb[pa]])
            yield
            rope(q3, QCb[pa][:], 64, 16, 12, 16, j, pa, [qcs[pa]], [QCb[pa]], rt[pa])
            yield
            kv3 = kvs[pa][:].rearrange("p (g k) -> p g k", k=128)
            P.cp("pool", KCb[pa][:, :, 0:64], kv3[:, :, 0:64], [kvs[pa]], [KCb[pa]])
            yield
            P.cp("act", vst[pa][:, 704:1024].rearrange("p (g k) -> p g k", k=64), kv3[:, :, 64:128], [kvs[pa]], [vst[pa]])
            yield
            P.cp("dve", KCb[pa][:, :, 64:96], bc(kr[pa][:].unsqueeze(1), [128, 5, 32]), [kr[pa]], [KCb[pa]])
            yield
            P.dma("sp", V_rows[j], vst[pa][:], [vst[pa]], [VALL])
            yield

        def s2c_post(j):
            pa = j % 2
            tC = pbf(T0)
            for h in range(5):
                P.tr(tC[0:96, h * 128:(h + 1) * 128], QCb[pa][:, h, :], identb[:], [QCb[pa], identb], [psum[T0]])
            P.cp("act", stC[pa][0:96, 0:5, :], tC[0:96, 0:640].rearrange("p (c t) -> p c t", t=128), [psum[T0]], [stC[pa]])
            tD = pbf(T1)
            for h in range(5):
                P.tr(tD[0:96, h * 128:(h + 1) * 128], KCb[pa][:, h, :], identb[:], [KCb[pa], identb], [psum[T1]])
            P.cp("dve", stC[pa][0:96, 5:10, :], tD[0:96, 0:640].rearrange("p (c t) -> p c t", t=128), [psum[T1]], [stC[pa]])
            P.dma("sp", QKC_v[:, :, j * 128:(j + 1) * 128], stC[pa][0:96, :, :], [stC[pa]], [QKC])

        load_x(0)
        load_x(1)
        for step in range(NT + 3):
            load_x(step + 2)
            if 0 <= step - 1 < NT:
                s1f(step - 1)
            gens = []
            if step < NT:
                gens.append(s0(step))
            if 0 <= step - 2 < NT:
                gens.append(s2a(step - 2))
            if 0 <= step - 3 < NT:
                gens.append(s2c_pre(step - 3))

            def feed(k, gens=gens):
                while k > 0 and gens:
                    try:
                        next(gens[0])
                        k -= 1
                    except StopIteration:
                        gens.pop(0)

            if 0 <= step - 1 < NT:
                s1b(step - 1, feed)
            feed(10 ** 6)
            if 0 <= step - 2 < NT:
                s2b(step - 2)
            if 0 <= step - 3 < NT:
                s2c_post(step - 3)

    def phase2(l):
        lam_init = 0.8 - 0.6 * math.exp(-0.3 * l)
        hg = P.sb([64, 12], F32)
        P.dma("sp", hg[:], hg_in[l, :, :], [hg_in], [hg])
        gB = P.sb([64, 1], F32)
        P.ts("dve", gB[:], hg[:, 11:12], 1.0 - lam_init, None, ALU.mult, ALU.bypass, [hg], [gB])
        lp = P.sb([64, 4, 32], F32)
        for i in range(4):
            P.dma("sp", lp[:, i, :], lam_in[l, i:i + 1, :].partition_broadcast(64), [lam_in], [lp])
        lpr = P.sb([64, 2, 32], F32)
        lsm = P.sb([64, 8], F32)
        P.tt("dve", lpr[:, 0, :], lp[:, 0, :], lp[:, 1, :], ALU.mult, [lp], [lpr])
        P.tt("dve", lpr[:, 1, :], lp[:, 2, :], lp[:, 3, :], ALU.mult, [lp], [lpr])
        P.op("dve", lambda e: e.tensor_reduce(out=lsm[:, 0:2], in_=lpr[:], axis=mybir.AxisListType.X, op=ALU.add), r=[lpr], w=[lsm])
        P.act(lsm[:, 2:4], lsm[:, 0:2], AF.Exp, [lsm], [lsm])
        P.tt("dve", lsm[:, 4:5], lsm[:, 3:4], lsm[:, 2:3], ALU.subtract, [lsm], [lsm])
        P.ts("dve", lsm[:, 5:6], lsm[:, 4:5], -lam_init, None, ALU.add, ALU.bypass, [lsm], [lsm])
        neglam = lsm[:, 5:6]
        m128 = P.sb([128, 64], F32)
        P.memset("dve", m128[0:64, :], 1.0 / 64, [m128])
        P.memset("dve", m128[64:128, :], 1e-6 / 64, [m128])
        amask = P.sb([128, 20, 512], BF16)
        P.dma("sp", amask[:], amask_in[:, :, :], [amask_in], [amask])

        QT = [P.sb([128, S], BF16) for _ in range(2)]
        KT = [P.sb([128, S], BF16) for _ in range(2)]
        VV = [P.sb([128, NT, 128], BF16) for _ in range(2)]
        for v in VV:
            P.memset("pool", v[:], 1.0, [v])
        Qz = [[P.sb([128, 512], BF16) for _ in range(2)] for _ in range(6)]
        for zz in Qz:
            for z in zz:
                P.memset("pool", z[:], 0.0, [z])
        Pt = [P.sb([128, 1536], BF16) for _ in range(4)]
        Pm = [P.sb([128, 1536], BF16) for _ in range(4)]
        fin = [[P.sb([128, 512], F32) for _ in range(5)] for _ in range(2)]
        yb = [P.sb([64, 512], BF16) for _ in range(2)]
        RB_ = 0
        fe = [[P.sb([128, 512], F32) for _ in range(2)] for _ in range(2)]
        V_v = VALL.ap().rearrange("(k p) c -> p k c", p=128)

        heads = [("A", h) for h in range(6)] + [("B", h) for h in range(5)] + [("C", h) for h in range(5)]
        blk_of = []
        for kind, h in heads:
            blk_of.append((kind, h if kind == "C" else h // 2))
        blk_seq = []
        for b_ in blk_of:
            if not blk_seq or blk_seq[-1] != b_:
                blk_seq.append(b_)
        blk_buf = {b_: i % 2 for i, b_ in enumerate(blk_seq)}
        loaded = set()

        def prefetch(hi):
            if hi >= len(heads):
                return
            kind, h = heads[hi]
            b_ = blk_of[hi]
            vv = VV[hi % 2]
            voff = {"A": 0, "B": 384, "C": 704}[kind] + h * 64
            P.dma("sp", vv[:, :, 0:64], V_v[:, :, voff:voff + 64], [VALL], [vv])
            if b_ in loaded:
                return
            loaded.add(b_)
            qt, kt_ = QT[blk_buf[b_]], KT[blk_buf[b_]]
            bi = b_[1]
            if kind == "A":
                P.dma("sp", qt[:, :], QKA[bi * 128:(bi + 1) * 128, :], [QKA], [qt])
                P.dma("sp", kt_[:, :], QKA[384 + bi * 128:384 + (bi + 1) * 128, :], [QKA], [kt_])
            elif kind == "B":
                nr = min(128, 320 - bi * 128)
                P.dma("sp", qt[0:nr, :], QKB[bi * 128:bi * 128 + nr, :], [QKB], [qt])
                P.dma("sp", kt_[0:nr, :], QKB[320 + bi * 128:320 + bi * 128 + nr, :], [QKB], [kt_])
            else:
                P.dma("sp", qt[0:96, :], QKC[bi, :, :], [QKC], [qt])
                P.dma("sp", kt_[0:96, :], QKC[5 + bi, :, :], [QKC], [kt_])

        def head(hi):
            kind, h = heads[hi]
            qt, kt_, vv = QT[blk_buf[blk_of[hi]]], KT[blk_buf[blk_of[hi]]], VV[hi % 2]
            prefetch(hi + 1)
            if kind == "A":
                dq, nmap, scale = 64, 1, 0.125
                rows = [(h % 2) * 64]
                zis = [4 + h % 2]
                orow, gcol = h * 64, hg[:, h:h + 1]
            elif kind == "B":
                dq, nmap, scale = 32, 2, 32 ** -0.5
                rows = [(h % 2) * 64, (h % 2) * 64 + 32]
                zis = [(h % 2) * 2, (h % 2) * 2 + 1]
                orow, gcol = 384 + h * 64, gB[:, 0:1]
            else:
                dq, nmap, scale = 96, 1, 96 ** -0.5
                rows, zis = [0], [None]
                orow, gcol = 704 + h * 64, hg[:, 6 + h:7 + h]

            def zcopy(qc):
                if kind == "C" or qc >= 8:
                    return
                for c in range(nmap):
                    z = Qz[zis[c]][qc % 2]
                    r0 = rows[c]
                    P.cp("pool", z[r0:r0 + dq, :], qt[r0:r0 + dq, qc * 512:(qc + 1) * 512], [qt], [z])

            units = []
            for qc in range(8):
                if kind == "A":
                    kts = [k for k in range(max(0, 4 * qc - 8), min(31, 4 * qc + 11) + 1)]
                else:
                    kts = list(range(NT))
                for ki, k in enumerate(kts):
                    for c in range(nmap):
                        units.append((qc, k, c, ki == 0, ki == len(kts) - 1))
            fin_q = []

            groups_ = []
            for qc in range(8):
                idxs = [i for i, u_ in enumerate(units) if u_[0] == qc]
                for g0 in range(0, len(idxs), 3):
                    groups_.append(idxs[g0:g0 + 3])

            def qk(p):
                sd = p % 2
                for u, i in enumerate(groups_[p]):
                    qc, k, c, first, last = units[i]
                    sb_ = psum[3 * sd + u]
                    if first and c == 0:
                        zcopy(qc + 1)
                    if kind == "C":
                        P.mm(sb_[:, :], kt_[0:96, k * 128:(k + 1) * 128], qt[0:96, qc * 512:(qc + 1) * 512], True, True, [kt_, qt], [sb_])
                    else:
                        z = Qz[zis[c]][qc % 2]
                        P.mm(sb_[:, :], kt_[:, k * 128:(k + 1) * 128], z[:, :], True, True, [kt_, z], [sb_])

            def ex(p):
                sd = p % 2
                g = groups_[p]
                n_ = len(g)
                qc, k, c, first, last = units[g[0]]
                bks = [psum[3 * sd + u] for u in range(n_)]
                P.act(Pt[p % 4][:, 0:n_ * 512], PSALL[:, sd * 1536:sd * 1536 + n_ * 512], AF.Exp, bks, [Pt[p % 4]], scale=scale)
                if kind == "A":
                    oi = (k * 128 - qc * 512 + 1024) // 128
                    P.tt("dve", Pm[p % 4][:, 0:n_ * 512], Pt[p % 4][:, 0:n_ * 512], amask[:, oi:oi + n_, :].rearrange("p a b -> p (a b)"), ALU.mult, [Pt[p % 4], amask], [Pm[p % 4]])

            def pv(p):
                for u, i in enumerate(groups_[p]):
                    qc, k, c, first, last = units[i]
                    ob = psum[6 + c] if kind == "B" else psum[6 + qc % 2]
                    src = Pm[p % 4] if kind == "A" else Pt[p % 4]
                    P.mm(ob[:, :], vv[:, k, :], src[:, u * 512:(u + 1) * 512], first, last, [vv, src], [ob])
                    if last and c == nmap - 1:
                        fin_q.append([qc, p + 3])
                        if kind == "B":
                            P.cp("dve", fe[qc % 2][0][:], psum[6][:, :], [psum[6]], [fe[qc % 2][0]])
                            P.cp("dve", fe[qc % 2][1][:], psum[7][:, :], [psum[7]], [fe[qc % 2][1]])

            def finalize1(qc):
                f = fin[qc % 2]
                if kind == "B":
                    o1, o2 = fe[qc % 2][0], fe[qc % 2][1]
                    P.op("dve", lambda e: e.reciprocal(out=f[0][0:64, :], in_=o1[64:128, :]), r=[o1], w=[f[0]])
                    P.op("dve", lambda e: e.reciprocal(out=f[1][0:64, :], in_=o2[64:128, :]), r=[o2], w=[f[1]])
                    P.tt("dve", f[2][0:64, :], o1[0:64, :], f[0][0:64, :], ALU.mult, [o1, f[0]], [f[2]])
                    P.stt(f[3][0:64, :], o2[0:64, :], neglam, f[1][0:64, :], ALU.mult, ALU.mult, [o2, f[1], lsm], [f[3]])
                    P.tt("pool", f[2][0:64, :], f[2][0:64, :], f[3][0:64, :], ALU.add, [f[2], f[3]], [f[2]])
                    P.tt("pool", f[4][0:64, :], f[2][0:64, :], f[2][0:64, :], ALU.mult, [f[2]], [f[4]])
                else:
                    o1 = psum[6 + qc % 2]
                    P.cp("dve", f[2][:], o1[:, :], [o1], [f[2]])
                    P.tt("pool", f[4][:], f[2][:], f[2][:], ALU.mult, [f[2]], [f[4]])

            def finalize2(qc):
                f = fin[qc % 2]
                y = yb[qc % 2]
                rb = psum[RB_]
                if kind == "B":
                    P.mm(rb[0:64, :], m128[0:64, :], f[4][0:64, :], True, True, [m128, f[4]], [rb])
                    P.act(f[0][0:64, :], rb[0:64, :], AF.Ln, [rb], [f[0]], bias=1e-5, scale=1.0)
                else:
                    P.mm(rb[0:64, :], m128[:, :], f[4][:], True, True, [m128, f[4]], [rb])
                    P.act(f[0][0:64, :], rb[0:64, :], AF.Ln, [rb], [f[0]])
                d_ = f[2]
                P.act(f[1][0:64, :], f[0][0:64, :], AF.Exp, [f[0]], [f[1]], scale=-0.5)
                P.stt(y[:], d_[0:64, :], gcol, f[1][0:64, :], ALU.mult, ALU.mult, [d_, f[1], hg, gB], [y])
                P.dma("sp", OT[orow:orow + 64, qc * 512:(qc + 1) * 512], y[:], [y], [OT])

            fin2_q = []

            def service(i):
                while fin_q and fin_q[0][1] <= i:
                    qc_ = fin_q.pop(0)[0]
                    finalize1(qc_)
                    fin2_q.append([qc_, i + (10 if kind == "B" else 4)])
                while fin2_q and fin2_q[0][1] <= i:
                    finalize2(fin2_q.pop(0)[0])

            n = len(groups_)
            zcopy(0)
            for i in range(n + 2):
                if i < n:
                    qk(i)
                if 0 <= i - 2 < n:
                    pv(i - 2)
                if i < n:
                    ex(i)
                service(i)
            while fin_q:
                qc_ = fin_q.pop(0)[0]
                finalize1(qc_)
                fin2_q.append([qc_, 0])
            while fin2_q:
                finalize2(fin2_q.pop(0)[0])

        prefetch(0)
        for hi in range(len(heads)):
            head(hi)

    def phase3(l, x_src):
        wout_bf = P.sb([128, 8, D], BF16)
        for c in range(8):
            P.dma("pool", wout_bf[:, c, :], wout_in[l, :, c, :], [wout_in], [wout_bf])
        g_ffn = P.sb([128, D], F32)
        P.dma("sp", g_ffn[:], grow_in[l, 1:2, :].partition_broadcast(128), [grow_in], [g_ffn])
        wr = P.sb([128, 8, NE], F32)
        P.dma("sp", wr[:], wr_in[l, :, :, :], [wr_in], [wr])
        aff = P.sb([128, NT, NE], F32)

        def mk(shape, dt):
            return [P.sb(shape, dt) for _ in range(2)]
        otg = mk([128, 8, 512], BF16)
        xt = [P.sb([128, D], F32) for _ in range(4)]; xm = mk([128, D], F32); junk = mk([128, D], BF16)
        hf = mk([128, D], F32); hb = mk([128, D], BF16); hT = mk([128, D], F32)
        st = mk([128, 8], F32); lg = mk([128, NE], F32)
        OT_v = OT.ap().rearrange("(c p) t -> p c t", p=128)
        x_rows = x_src.ap().rearrange("(j p) d -> j p d", p=128)
        XR_rows = XR.ap().rearrange("(j p) d -> j p d", p=128)
        H_rows = H.ap().rearrange("(j p) d -> j p d", p=128)
        st2 = mk([128, 8], F32)
        lgT = mk([NE, 128], F32)

        def p3_load(j):
            if j >= NT:
                return
            if j % 4 == 0:
                g4_ = (j // 4) % 2
                P.dma("sp", otg[g4_][:], OT_v[:, :, j * 128:j * 128 + 512], [OT], [otg[g4_]])
            P.dma("sp", xt[j % 4][:], x_rows[j], [x_src], [xt[j % 4]])

        def p3a(j):
            pa = j % 2
            g4 = (j // 4) % 2
            tq = (j % 4) * 128
            for half in range(2):
                bk = psum[half]
                for c in range(8):
                    P.mm(bk[:, :], otg[g4][:, c, tq:tq + 128], wout_bf[:, c, half * 512:(half + 1) * 512], c == 0, c == 7, [otg[g4], wout_bf], [bk])
                P.tt("dve", xm[pa][:, half * 512:(half + 1) * 512], bk[:, :], xt[j % 4][:, half * 512:(half + 1) * 512], ALU.add, [bk, xt[j % 4]], [xm[pa]])
            P.dma("sp", XR_rows[j], xm[pa][:], [xm[pa]], [XR])
            s_ = st[pa]
            P.act(junk[pa][:], xm[pa][:], AF.Square, [xm[pa]], [junk[pa], s_], accum_out=s_[:, 0:1])
            P.act(s_[:, 1:2], s_[:, 0:1], AF.Ln, [s_], [s_], scale=1.0 / D, bias=1e-6)
            P.act(s_[:, 2:3], s_[:, 1:2], AF.Exp, [s_], [s_], scale=-0.5)
            P.stt(hf[pa][:], xm[pa][:], s_[:, 2:3], g_ffn[:], ALU.mult, ALU.mult, [xm[pa], s_, g_ffn], [hf[pa]])
            P.cp("pool", hb[pa][:], hf[pa][:], [hf[pa]], [hb[pa]])
            P.dma("sp", H_rows[j], hb[pa][:], [hb[pa]], [H])

        def p3b(j):
            pa = j % 2
            s_ = st2[pa]
            for c in range(8):
                bk = psum[2 + c // 4]
                P.tr(bk[:, (c % 4) * 128:(c % 4 + 1) * 128], hf[pa][:, c * 128:(c + 1) * 128], identf[:], [hf[pa], identf], [bk])
            P.cp("act", hT[pa][:, 0:512], psum[2][:, :], [psum[2]], [hT[pa]])
            P.cp("dve", hT[pa][:, 512:1024], psum[3][:, :], [psum[3]], [hT[pa]])
            lb = psum[4 + pa]
            lt = psum[6 + pa]
            for c in range(8):
                P.mm(lt[0:NE, 0:128], wr[:, c, :], hT[pa][:, c * 128:(c + 1) * 128], c == 0, c == 7, [hT[pa], wr], [lt])
            P.cp("act", lgT[pa][:], lt[0:NE, 0:128], [lt], [lgT[pa]])
            P.tr(lb[:, 0:NE], lgT[pa][:], identf[0:NE, 0:NE], [lgT[pa], identf], [lb])
            P.act(lg[pa][:], lb[:, 0:NE], AF.Exp, [lb], [lg[pa], s_], accum_out=s_[:, 3:4])
            P.op("dve", lambda e, a=s_[:, 4:5], b=s_[:, 3:4]: e.reciprocal(out=a, in_=b), r=[s_], w=[s_])
            P.ts("dve", aff[:, j, :], lg[pa][:], s_[:, 4:5], None, ALU.mult, ALU.bypass, [lg[pa], s_], [aff])

        p3_load(0)
        p3_load(1)
        for step in range(NT + 1):
            p3_load(step + 2)
            if step < NT:
                p3a(step)
            if 0 <= step - 1 < NT:
                p3b(step - 1)
        P.dma("sp", AFF[:, :], aff[:].rearrange("p j e -> p (j e)"), [aff], [AFF])

    def phase4(l):
        aff = P.sb([128, NT, NE], F32)
        P.dma("sp", aff[:].rearrange("p j e -> p (j e)"), AFF[:, :], [AFF], [aff])
        aff8 = P.sb([128, 4, 128], F32)
        for t in range(4):
            bk = psum[t % 2]
            P.tr(bk[:, 0:128], aff[:, 8 * t:8 * t + 8, :].rearrange("p j e -> p (j e)"), identf[:], [aff, identf], [bk])
            P.cp("act" if t % 2 else "dve", aff8[:, t, :], bk[:, 0:128], [bk], [aff8])
        gsum = P.sb([128, 128], F32)
        P.dma("sp", gsum[:], gsum_in[:, :], [gsum_in], [gsum])
        lo = P.sb([128, 1], F32); mid = P.sb([128, 1], F32); cnt = P.sb([128, 2], F32); sel = P.sb([128, 1], F32)
        junk = P.sb([128, 512], BF16)
        P.memset("dve", lo[:], 0.0, [lo])
        P.memset("dve", cnt[:], 0.0, [cnt])
        a8 = aff8[:].rearrange("p t q -> p (t q)")
        for it in range(30):
            w_ = 0.5 ** (it + 1)
            P.ts("dve", mid[:], lo[:], w_, None, ALU.add, ALU.bypass, [lo], [mid])
            P.ts("dve", junk[:], a8, mid[:, 0:1], 0.0, ALU.is_ge, ALU.add, [aff8, mid], [junk, cnt], accum_out=cnt[:, 0:1])
            P.mm(psum[2][:, 0:2], gsum[:], cnt[:, 0:2], True, True, [gsum, cnt], [psum[2]])
            P.ts("dve", sel[:], psum[2][:, 0:1], float(CAP) - 0.5, w_, ALU.is_ge, ALU.mult, [psum[2]], [sel])
            P.tt("dve", lo[:], lo[:], sel[:], ALU.add, [lo, sel], [lo])
        P.dma("sp", THR[:, :], lo[0:NE, :], [lo], [THR])
        thr = P.sb([128, NE], F32)
        P.dma("sp", thr[:], THR.ap().rearrange("e o -> o e").partition_broadcast(128), [THR], [thr])
        mk_ = P.sb([128, NT, NE], F32)
        mkb = P.sb([128, NT, NE], BF16)
        P.tt("dve", mk_[:], aff[:], bc(thr[:].unsqueeze(1), [128, NT, NE]), ALU.is_ge, [aff, thr], [mk_])
        P.cp("dve", mkb[:], mk_[:], [mk_], [mkb])
        tri = P.sb([128, 128], BF16); onesb = P.sb([128, 128], BF16)
        P.dma("sp", tri[:], tri_in[:, :], [tri_in], [tri])
        P.memset("dve", onesb[:], 1.0, [onesb])
        mkb2 = mkb[:].rearrange("p j e -> p (j e)")
        P.mm(psum[2][:, :], tri[:], mkb2, True, True, [tri, mkb], [psum[2]])
        P.mm(psum[3][:, :], onesb[:], mkb2, True, True, [onesb, mkb], [psum[3]])
        cntt = P.sb([128, NT, NE], F32); carry = P.sb([128, NT, NE], F32); posm = P.sb([128, NT, NE], F32)
        P.cp("act", cntt[:].rearrange("p j e -> p (j e)"), psum[3][:, :], [psum[3]], [cntt])
        P.memset("dve", carry[:, 0, :], 0.0, [carry])
        for j in range(1, NT):
            P.tt("dve", carry[:, j, :], carry[:, j - 1, :], cntt[:, j - 1, :], ALU.add, [carry, cntt], [carry])
        P.tt("dve", posm[:].rearrange("p j e -> p (j e)"), psum[2][:, :], carry[:].rearrange("p j e -> p (j e)"), ALU.add, [psum[2], carry], [posm])
        P.tt("dve", posm[:], posm[:], mk_[:], ALU.mult, [posm, mk_], [posm])
        P.ts("dve", posm[:], posm[:], -1.0, None, ALU.add, ALU.bypass, [posm], [posm])
        rhsE = P.sb([128, NT, NE, 6], BF16)
        P.memset("dve", rhsE[:], 0.0, [rhsE])
        tokab = P.sb([128, NT, 2], BF16)
        P.dma("sp", tokab[:], tokab_in[:, :, :], [tokab_in], [tokab])
        P.cp("dve", rhsE[:, :, :, 0:2], bc(tokab[:].unsqueeze(2), [128, NT, NE, 2]), [tokab], [rhsE])
        r1 = P.sb([128, NT, NE], F32); gp = P.sb([128, NT, NE], BF16); gpf = P.sb([128, NT, NE], F32)
        P.cp("dve", r1[:], aff[:], [aff], [r1])
        for k in range(3):
            P.cp("dve", gp[:], r1[:], [r1], [gp])
            P.cp("dve", rhsE[:, :, :, 2 + k:3 + k], gp[:].unsqueeze(3), [gp], [rhsE])
            if k < 2:
                P.cp("dve", gpf[:], gp[:], [gp], [gpf])
                P.tt("dve", r1[:], r1[:], gpf[:], ALU.subtract, [r1, gpf], [r1])
        iota = P.sb([128, CAP], mybir.dt.float16)
        P.dma("sp", iota[:], iota_in[:, :], [iota_in], [iota])
        OH = [P.sb([128, CAP], BF16) for _ in range(4)]
        RT = P.sb([6, NE, CAP], F32)
        k = 0
        for e in range(NE):
            bk = psum[4 + e % 2]
            for j in range(NT):
                oh = OH[k % 4]
                P.ts("dve", oh[:], iota[:], posm[:, j, e:e + 1], None, ALU.is_equal, ALU.bypass, [iota, posm], [oh])
                P.mm(bk[0:6, :], rhsE[:, j, e, :], oh[:], j == 0, j == NT - 1, [rhsE, oh], [bk])
                k += 1
            P.cp("act", RT[:, e, :], bk[0:6, :], [bk], [RT])
        IDXG = P.sb([128, 64, 6], F32)
        for e in range(NE):
            for c in range(4):
                i = e * 4 + c
                P.tr(psum[6][:, i * 6:(i + 1) * 6], RT[:, e, c * 128:(c + 1) * 128], identf[0:6, 0:6], [RT, identf], [psum[6]])
        P.cp("dve", IDXG[:].rearrange("p i k -> p (i k)"), psum[6][:, 0:384], [psum[6]], [IDXG])
        ig = P.sb([128, 64, 2], F32)
        P.stt(ig[:, :, 0:1], IDXG[:, :, 0:1], 64.0, IDXG[:, :, 1:2], ALU.mult, ALU.add, [IDXG], [ig])
        P.tt("dve", ig[:, :, 1:2], IDXG[:, :, 2:3], IDXG[:, :, 3:4], ALU.add, [IDXG], [ig])
        P.tt("dve", ig[:, :, 1:2], ig[:, :, 1:2], IDXG[:, :, 4:5], ALU.add, [IDXG, ig], [ig])
        P.dma("sp", IDXD[:, :], ig[:].rearrange("p i k -> p (i k)"), [ig], [IDXD])

    def phase5(l):
        ig = P.sb([128, 64, 2], F32)
        P.dma("sp", ig[:].rearrange("p i k -> p (i k)"), IDXD[:, :], [IDXD], [ig])
        idx = P.sb([128, 64], I32)
        gate = P.sb([128, 64], F32)
        P.cp("dve", idx[:], ig[:, :, 0], [ig], [idx])
        P.cp("dve", gate[:], ig[:, :, 1], [ig], [gate])
        G = [[P.sb([128, D], BF16) for _ in range(4)] for _ in range(2)]
        xeT = [P.sb([128, 8, CAP], BF16) for _ in range(2)]
        gT = [P.sb([128, NF, CAP], BF16) for _ in range(2)]
        WGb = [P.sb([128, 2, D], BF16) for _ in range(4)]
        WUb = [P.sb([128, 2, D], BF16) for _ in range(4)]
        WDb = [P.sb([128, NF, D], BF16) for _ in range(2)]
        sa = [P.sb([128, CAP], F32) for _ in range(2)]
        yo = [P.sb([128, D], F32) for _ in range(4)]
        groups = [(0, 2), (2, 2), (4, 2), (6, 2), (8, 2), (10, 1)]
        glist = [(e, gi) for e in range(NE) for gi in range(6)]

        def load_group(n):
            if n >= len(glist):
                return
            e, gi = glist[n]
            f0, nf = groups[gi]
            b = n % 4
            P.dma("pool", WGb[b][:, 0:nf, :], wg_in[l, e, f0:f0 + nf, :, :].rearrange("f p d -> p f d"), [wg_in], [WGb[b]])
            P.dma("pool", WUb[b][:, 0:nf, :], wu_in[l, e, f0:f0 + nf, :, :].rearrange("f p d -> p f d"), [wu_in], [WUb[b]])

        def load_wd(e):
            if e >= NE:
                return
            for f0, nf in ((0, 4), (4, 4), (8, 3)):
                P.dma("pool", WDb[e % 2][:, f0:f0 + nf, :], wd_in[l, e, f0 * 128:(f0 + nf) * 128, :].rearrange("(f p) d -> p f d", p=128), [wd_in], [WDb[e % 2]])

        def gathers(e):
            if e >= NE:
                return
            for c in range(4):
                i = e * 4 + c
                P.op("pool", lambda en, o=G[e % 2][c], ix=idx[:, i:i + 1]: en.indirect_dma_start(
                    out=o[:], out_offset=None, in_=H[:, :], in_offset=bass.IndirectOffsetOnAxis(ap=ix, axis=0)),
                    r=[idx, H], w=[G[e % 2][c]], dma=True)

        def pass2_block(e, c, half):
            eb = e % 2
            i = e * 4 + c
            bk = psum[6 + half]
            for f in range(NF):
                P.mm(bk[:, :], gT[eb][:, f, c * 128:(c + 1) * 128], WDb[eb][:, f, half * 512:(half + 1) * 512], f == 0, f == NF - 1, [gT[eb], WDb[eb]], [bk])
            if half == 0:
                P.ts("dve", yo[c][:, 0:512], bk[:, :], gate[:, i:i + 1], None, ALU.mult, ALU.bypass, [bk, gate], [yo[c]])
            else:
                P.act(yo[c][:, 512:1024], bk[:, :], AF.Copy, [bk, gate], [yo[c]], scale=gate[:, i:i + 1])
                P.op("pool", lambda en, o=yo[c], ix=idx[:, i:i + 1]: en.indirect_dma_start(
                    out=XR[:, :], out_offset=bass.IndirectOffsetOnAxis(ap=ix, axis=0), in_=o[:], in_offset=None, compute_op=ALU.add),
                    r=[idx, yo[c], XR], w=[XR], dma=True)

        gathers(0)
        load_group(0)
        load_group(1)
        load_group(2)
        load_wd(0)
        n = 0
        k = 0
        pend = []
        for e in range(NE):
            eb = e % 2
            gathers(e + 1)
            for c in range(4):
                tb = pbf(c % 2)
                for dc in range(8):
                    P.tr(tb[:, dc * 128:(dc + 1) * 128], G[eb][c][:, dc * 128:(dc + 1) * 128], identb[:], [G[eb][c], identb], [psum[c % 2]])
                P.cp("dve" if c % 2 else "act", xeT[eb][:, :, c * 128:(c + 1) * 128], tb[:, 0:D].rearrange("p (d t) -> p d t", t=128), [psum[c % 2]], [xeT[eb]])
            for gi in range(6):
                load_group(n + 3)
                f0, nf = groups[gi]
                b = n % 4
                n += 1
                for fi in range(nf):
                    f = f0 + fi
                    fb = k % 2
                    k += 1
                    pa_, pu_ = psum[2 + fb], psum[4 + fb]
                    for dc in range(8):
                        P.mm(pa_[:, :], WGb[b][:, fi, dc * 128:(dc + 1) * 128], xeT[eb][:, dc, :], dc == 0, dc == 7, [WGb[b], xeT[eb]], [pa_])
                    for dc in range(8):
                        P.mm(pu_[:, :], WUb[b][:, fi, dc * 128:(dc + 1) * 128], xeT[eb][:, dc, :], dc == 0, dc == 7, [WUb[b], xeT[eb]], [pu_])
                    P.act(sa[fb][:], pa_[:, :], AF.Silu, [pa_], [sa[fb]])
                    P.tt("dve", gT[eb][:, f, :], sa[fb][:], pu_[:, :], ALU.mult, [sa[fb], pu_], [gT[eb]])
                for _ in range(2):
                    if pend:
                        pass2_block(*pend.pop(0))
            while pend:
                pass2_block(*pend.pop(0))
            load_wd(e + 1)
            pend = [(e, c, half) for c in range(4) for half in range(2)]
        while pend:
            pass2_block(*pend.pop(0))

    def phase_final():
        g_f = P.sb([128, D], F32)
        P.dma("sp", g_f[:], fing_in[0:1, :].partition_broadcast(128), [fing_in], [g_f])
        NB = 6
        xt = [P.sb([128, D], F32) for _ in range(NB)]
        junk = [P.sb([128, D], BF16) for _ in range(2)]
        yo = [P.sb([128, D], F32) for _ in range(4)]
        st = [P.sb([128, 4], F32) for _ in range(4)]
        XR_rows = XR.ap().rearrange("(j p) d -> j p d", p=128)
        O_rows = out_d.ap().rearrange("(j p) d -> j p d", p=128)
        for j in range(min(NB - 1, NT)):
            P.dma("sp", xt[j % NB][:], XR_rows[j], [XR], [xt[j % NB]])
        for j in range(NT):
            if j + NB - 1 < NT:
                jj = j + NB - 1
                P.dma("sp", xt[jj % NB][:], XR_rows[jj], [XR], [xt[jj % NB]])
            x_ = xt[j % NB]
            s_ = st[j % 4]
            y_ = yo[j % 4]
            P.act(junk[j % 2][:], x_[:], AF.Square, [x_], [junk[j % 2], s_], accum_out=s_[:, 0:1])
            P.act(s_[:, 1:2], s_[:, 0:1], AF.Ln, [s_], [s_], scale=1.0 / D, bias=1e-6)
            P.act(s_[:, 2:3], s_[:, 1:2], AF.Exp, [s_], [s_], scale=-0.5)
            P.stt(y_[:], x_[:], s_[:, 2:3], g_f[:], ALU.mult, ALU.mult, [x_, s_, g_f], [y_])
            P.dma("pool", O_rows[j], y_[:], [y_], [out_d])

    return P, dict(phase0=phase0, p1=phase1, p2=phase2, p3=phase3, p4=phase4, p5=phase5, final=phase_final,
                   x_in=x_in, XR=XR)


def build(n_layers=DEPTH, upto="all", debug=False):
    import contextlib
    with contextlib.ExitStack() as es_glob:
        return _build(es_glob, n_layers, upto, debug)


def _build(es_glob, n_layers, upto, debug):
    import contextlib
    P, ph = _define(es_glob, debug)
    nc = P.nc

    def run(fn, *a):
        with contextlib.ExitStack() as es:
            P.es = es
            fn(*a)
            P.barrier()
            P.emit()
        P.es = es_glob

    run(ph["phase0"])
    done = False
    for l in range(n_layers):
        x_src = ph["x_in"] if l == 0 else ph["XR"]
        for name in ["p1", "p2", "p3", "p4", "p5"]:
            if name in ("p1", "p3"):
                run(ph[name], l, x_src)
            else:
                run(ph[name], l)
            if upto != "all" and (l, name) == tuple(upto):
                done = True
                break
        if done:
            break
    if upto == "all":
        run(ph["final"])
    return nc


def _consts():
    c = {}
    c["identb"] = np.eye(128, dtype=np.float32).astype(ml_dtypes.bfloat16)
    c["identf"] = np.eye(128, dtype=np.float32)
    fa = 1.0 / (500000.0 ** (np.arange(0, 16, 2, dtype=np.float32) / 16))
    fb = 1.0 / (500000.0 ** (np.arange(0, 8, 2, dtype=np.float32) / 8))
    fc = 1.0 / (10000.0 ** (np.arange(0, 32, 2, dtype=np.float32) / 32))
    inv = np.concatenate([fa, fb, fc]).astype(np.float64) / (2 * np.pi)
    c["invf"] = np.tile(inv.astype(np.float32)[None, :], (128, 1))
    i = np.arange(128)[:, None]
    jq = np.arange(512)[None, :]
    am = np.zeros((128, 20, 512), np.float32)
    for oi in range(20):
        o = oi * 128 - 1024
        dl = o + i - jq
        a = np.abs(dl)
        am[:, oi, :] = (a <= 64).astype(np.float32) + ((dl % 4 == 0) & (a <= 256)) + ((dl % 16 == 0) & (a <= 1024))
    c["amask"] = am.astype(ml_dtypes.bfloat16)
    c["gsum"] = (np.arange(128)[:, None] % 16 == np.arange(128)[None, :] % 16).astype(np.float32)
    c["tri"] = (np.arange(128)[:, None] <= np.arange(128)[None, :]).astype(np.float32).astype(ml_dtypes.bfloat16)
    c["iota"] = np.tile(np.arange(512, dtype=np.float16)[None, :], (128, 1))
    t = np.arange(NT)[None, :] * 128 + np.arange(128)[:, None]
    c["tokab"] = np.stack([t // 64, t % 64], axis=-1).astype(np.float32).astype(ml_dtypes.bfloat16)
    return c


def prep_inputs(x, positions, attn_norm_g, w_in, lam_q1, lam_k1, lam_q2, lam_k2, diff_subln_g,
                mla_q_norm_g, mla_w_uq, mla_kv_norm_g, mla_w_ukv, dil_out_g, mla_out_g, w_out,
                ffn_norm_g, w_router, w_gate, w_up, w_down, final_norm_g):
    f = lambda a: np.ascontiguousarray(np.asarray(a, dtype=np.float32))
    sh = dict(_consts())
    sh["win"] = f(np.asarray(w_in).reshape(DEPTH, 8, 128, INW).transpose(0, 2, 1, 3))
    sh["wout"] = f(np.asarray(w_out).reshape(DEPTH, 8, 128, D).transpose(0, 2, 1, 3))
    sh["wuq"] = f(mla_w_uq)
    sh["wukv"] = f(mla_w_ukv)
    sh["wr"] = f(np.asarray(w_router).reshape(DEPTH, 8, 128, NE).transpose(0, 2, 1, 3))
    sh["wg"] = f(np.asarray(w_gate).reshape(DEPTH, NE, 8, 128, NF, 128).transpose(0, 1, 4, 3, 2, 5).reshape(DEPTH, NE, NF, 128, D))
    sh["wu"] = f(np.asarray(w_up).reshape(DEPTH, NE, 8, 128, NF, 128).transpose(0, 1, 4, 3, 2, 5).reshape(DEPTH, NE, NF, 128, D))
    sh["wd"] = f(w_down)
    sh["grow"] = f(np.stack([np.asarray(attn_norm_g), np.asarray(ffn_norm_g)], axis=1))
    sh["fing"] = f(np.asarray(final_norm_g).reshape(1, D))
    sh["qng"] = f(mla_q_norm_g)
    sh["kvng"] = f(mla_kv_norm_g)
    hg = np.zeros((DEPTH, 64, 12), np.float32)
    hg[:, :, 0:6] = np.asarray(dil_out_g).reshape(DEPTH, 6, 64).transpose(0, 2, 1)
    hg[:, :, 6:11] = np.asarray(mla_out_g).reshape(DEPTH, 5, 64).transpose(0, 2, 1)
    hg[:, :, 11] = np.asarray(diff_subln_g)
    sh["hg"] = hg
    sh["lam"] = f(np.stack([np.asarray(lam_q1), np.asarray(lam_k1), np.asarray(lam_q2), np.asarray(lam_k2)], axis=1))
    xs = np.asarray(x, dtype=np.float32)
    ps = np.asarray(positions).astype(np.int32)
    in_maps = []
    for b in range(8):
        m = dict(sh)
        m["x"] = np.ascontiguousarray(xs[b])
        m["pos"] = np.ascontiguousarray(ps[b].reshape(NT, 128).T)
        in_maps.append(m)
    return in_maps


_NC_CACHE = {}


def kernel(**inputs):
    in_maps = prep_inputs(**inputs)
    if "nc" not in _NC_CACHE:
        _NC_CACHE["nc"] = build()
    res = run_bass_kernel_spmd(_NC_CACHE["nc"], in_maps, core_ids=list(range(8)))
    return np.stack([np.asarray(r["out"], dtype=np.float32) for r in res.results], axis=0)
```
